# Optimizing a Trainium2 kernel written in Bass

```python
import math
import functools
import jax
import jax.numpy as jnp
from jax import lax
import numpy as np

D_MODEL = 2048
BATCH = 8
SEQ = 2048
DEPTH = 2

GRID_W = 64
CTX_LEN = 256
HEAD_DIM = 128
N_HEADS = D_MODEL // HEAD_DIM
NA_HEADS = N_HEADS // 2
NA_WIN_H = 8
NA_WIN_W = 16
NA_KCOLS = 2 * NA_WIN_W
RET_HEADS = N_HEADS // 2
RET_DK = HEAD_DIM
RET_DV = 2 * HEAD_DIM
RET_CHUNK = 128
DIFF_HEADS = N_HEADS // 2
DIFF_DK = HEAD_DIM // 2
DIFF_DV = HEAD_DIM
HG_HEADS = N_HEADS // 2
HG_DK = HEAD_DIM
HG_DV = HEAD_DIM
HG_CHUNK = 64
Q_BLOCK = 128
N_GROUPS = 4
EXPERTS_PER_GROUP = 8
N_EXPERTS = N_GROUPS * EXPERTS_PER_GROUP
TOP_K = 2
EXPERT_FF = D_MODEL // 2
MOE_BLOCK = 128
ROPE_BASE = 10000.0
EPS = 1e-6
N_EVEN = (DEPTH + 1) // 2
N_ODD = DEPTH // 2
F32 = jnp.float32
EVEN_IN_SIZES = (NA_HEADS * HEAD_DIM,) * 3 + (RET_HEADS * RET_DK,) * 2 + (RET_HEADS * RET_DV,) * 2
ODD_IN_SIZES = (DIFF_HEADS * 2 * DIFF_DK,) * 2 + (DIFF_HEADS * DIFF_DV,) + (HG_HEADS * HG_DK,) * 3 + (HG_HEADS * HG_DV,) * 2
EVEN_MIX = NA_HEADS * HEAD_DIM + RET_HEADS * RET_DV
ODD_MIX = DIFF_HEADS * DIFF_DV + HG_HEADS * HG_DV

kernel_name = 'hybrid_natten_retnet_diffattn_hgrn2_hmoe_dit'


def rms_norm(x, w):
    xf = x.astype(F32)
    y = xf * lax.rsqrt(jnp.mean(xf * xf, axis=-1, keepdims=True) + EPS)
    return y.astype(x.dtype) * w


def head_rms(o):
    of = o.astype(F32)
    return (of * lax.rsqrt(jnp.mean(of * of, axis=-1, keepdims=True) + EPS)).astype(o.dtype)


def split_cols(t, sizes):
    cuts = [int(s) for s in np.cumsum(sizes)[:-1]]
    return jnp.split(t, cuts, axis=-1)


def heads(t, n):
    b, s, _ = t.shape
    return t.reshape(b, s, n, -1).transpose(0, 2, 1, 3)


def merge(o):
    b, h, s, d = o.shape
    return o.transpose(0, 2, 1, 3).reshape(b, s, h * d)


def diff_heads(t):
    b, s, _ = t.shape
    return t.reshape(b, s, DIFF_HEADS, 2, DIFF_DK).transpose(0, 2, 3, 1, 4)


def rope_tables(n_tok, dim):
    n_f = dim // 4
    inv = ROPE_BASE ** (-jnp.arange(n_f, dtype=F32) / n_f)
    t = jnp.arange(n_tok)
    pos = jnp.stack([t // GRID_W, t % GRID_W], axis=-1).astype(F32)
    ang = pos[:, :, None] * inv
    return jnp.cos(ang), jnp.sin(ang)


def apply_rope(x, cos, sin):
    n_f = cos.shape[-1]
    x5 = x.reshape(x.shape[:-1] + (2, 2, n_f))
    a, b = x5[..., 0, :], x5[..., 1, :]
    out = jnp.stack([a * cos - b * sin, b * cos + a * sin], axis=-2)
    return out.reshape(x.shape).astype(x.dtype)


def softmax_attention(q, k, v):
    s = jnp.einsum('bhqd,bhkd->bhqk', q * q.shape[-1] ** -0.5, k).astype(F32)
    p = jax.nn.softmax(s, axis=-1).astype(v.dtype)
    return jnp.einsum('bhqk,bhkd->bhqd', p, v)


def neighborhood_attention(q, k, v, k_ctx, v_ctx, rpb):
    b, h, n_tok, d = q.shape
    rows = n_tok // GRID_W
    kh = min(NA_WIN_H, rows)
    kw = NA_WIN_W
    ncb = GRID_W // kw
    qg = (q * d ** -0.5).reshape(b, h, rows, GRID_W, d)
    kg = k.reshape(b, h, rows, GRID_W, d)
    vg = v.reshape(b, h, rows, GRID_W, d)
    r_start = jnp.clip(jnp.arange(rows) - kh // 2, 0, rows - kh)
    cidx = jnp.arange(GRID_W)
    c_start = jnp.clip(cidx - kw // 2, 0, GRID_W - kw)
    qcol = cidx.reshape(ncb, kw)
    blk_start = jnp.minimum(c_start[qcol[:, 0]], GRID_W - NA_KCOLS)
    kcol = blk_start[:, None] + jnp.arange(NA_KCOLS)
    qs = c_start[qcol][:, :, None]
    kc3 = kcol[:, None, :]
    col_mask = (kc3 >= qs) & (kc3 < qs + kw)
    col_off = jnp.clip(kc3 - qcol[:, :, None] + NA_WIN_W - 1, 0, 2 * NA_WIN_W - 2)
    rpb_c = rpb.astype(F32)[:, :, col_off]
    n_loc = kh * NA_KCOLS

    def row(r):
        rs = r_start[r]
        kb = lax.dynamic_slice_in_dim(kg, rs, kh, axis=2)[:, :, :, kcol]
        vb = lax.dynamic_slice_in_dim(vg, rs, kh, axis=2)[:, :, :, kcol]
        qr = lax.dynamic_index_in_dim(qg, r, axis=2, keepdims=False).reshape(b, h, ncb, kw, d)
        row_off = rs + jnp.arange(kh) - r + NA_WIN_H - 1
        bias = jnp.transpose(rpb_c[:, row_off], (0, 2, 3, 1, 4))
        s_loc = jnp.einsum('bhnqd,bhinkd->bhnqik', qr, kb).astype(F32) + bias
        s_loc = jnp.where(col_mask[:, :, None, :], s_loc, -jnp.inf)
        s_ctx = jnp.einsum('bhnqd,bhcd->bhnqc', qr, k_ctx).astype(F32)
        s = jnp.concatenate([s_loc.reshape(b, h, ncb, kw, n_loc), s_ctx], axis=-1)
        p = jax.nn.softmax(s, axis=-1).astype(v.dtype)
        o = jnp.einsum('bhnqik,bhinkd->bhnqd', p[..., :n_loc].reshape(b, h, ncb, kw, kh, NA_KCOLS), vb)
        o = o + jnp.einsum('bhnqc,bhcd->bhnqd', p[..., n_loc:], v_ctx)
        return o.reshape(b, h, GRID_W, d)

    o = lax.map(row, jnp.arange(rows))
    return jnp.transpose(o, (1, 2, 0, 3, 4)).reshape(b, h, n_tok, d)


def retention_scan(q, k, v, s0, log_g):
    b, h, n_tok, _ = q.shape
    dv = v.shape[-1]
    c = RET_CHUNK
    n = n_tok // c
    qf, kf, vf = (t.astype(F32).reshape(b, h, n, c, -1) for t in (q, k, v))
    pos = jnp.arange(c, dtype=F32)
    lg = log_g.astype(F32)[:, None, None]
    rel = pos[:, None] - pos[None, :]
    dmask = jnp.where(rel >= 0, jnp.exp(jnp.maximum(rel, 0.0) * lg), 0.0)
    scores = jnp.einsum('bhnid,bhnjd->bhnij', qf, kf) * dmask[:, None]
    intra = jnp.einsum('bhnij,bhnje->bhnie', scores, vf)
    q_dec = qf * jnp.exp(lg * (pos + 1.0))[..., None]
    k_dec = kf * jnp.exp(lg * (c - 1.0 - pos))[..., None]
    kv = jnp.einsum('bhncd,bhnce->nbhde', k_dec, vf)
    g_chunk = jnp.exp(log_g.astype(F32) * c)[None, :, None, None]

    def step(s, kv_i):
        return s * g_chunk + kv_i, s

    s_fin, s_prev = lax.scan(step, s0, kv)
    inter = jnp.einsum('bhncd,nbhde->bhnce', q_dec, s_prev)
    return (intra + inter).reshape(b, h, n_tok, dv).astype(q.dtype), s_fin


def hgrn_scan(q, k, v, logf, s0):
    b, h, n_tok, _ = q.shape
    dv = v.shape[-1]
    c = HG_CHUNK
    n = n_tok // c
    prep = lambda t: jnp.moveaxis(t.astype(F32).reshape(b, h, n, c, -1), 2, 0)
    tril = jnp.tril(jnp.ones((c, c), dtype=bool))

    def step(s, inp):
        qc, kc, vc, lf = inp
        cum = jnp.cumsum(lf, axis=2)
        inter = jnp.einsum('bhcd,bhde->bhce', qc * jnp.exp(cum), s)
        diff = cum[:, :, :, None, :] - cum[:, :, None, :, :]
        dec = jnp.exp(jnp.where(tril[:, :, None], diff, -jnp.inf))
        att = jnp.einsum('bhid,bhjd,bhijd->bhij', qc, kc, dec)
        intra = jnp.einsum('bhij,bhje->bhie', att, vc)
        last = cum[:, :, -1:, :]
        s_new = jnp.exp(last[:, :, 0])[..., None] * s + jnp.einsum('bhjd,bhje->bhde', kc * jnp.exp(last - cum), vc)
        return s_new, inter + intra

    s_fin, o = lax.scan(step, s0, (prep(q), prep(k), prep(v), prep(logf)))
    return jnp.moveaxis(o, 0, 2).reshape(b, h, n_tok, dv).astype(q.dtype), s_fin


def bidirectional(scan_f, scan_b, lat_f, ctx_f, lat_b, ctx_b, s0, with_ctx):
    flip = lambda ts: tuple(jnp.flip(t, axis=2) for t in ts)
    oc_f, sc_f = scan_f(*ctx_f, s0)
    ol_f, _ = scan_f(*lat_f, sc_f)
    oc_b, sc_b = scan_b(*flip(ctx_b), s0)
    ol_b, _ = scan_b(*flip(lat_b), sc_b)
    lat = ol_f + jnp.flip(ol_b, axis=2)
    ctx = oc_f + jnp.flip(oc_b, axis=2) if with_ctx else None
    return lat, ctx


def diff_attention(q, k, v, lam):
    b, h, _, tq, dk = q.shape
    nb = tq // Q_BLOCK
    qb = jnp.moveaxis(q.reshape(b, h, 2, nb, Q_BLOCK, dk), 3, 0)

    def blk(qi):
        s = jnp.einsum('bhtqd,bhtkd->bhtqk', qi, k).astype(F32)
        p = jax.nn.softmax(s, axis=-1)
        a = (p[:, :, 0] - lam * p[:, :, 1]).astype(v.dtype)
        return jnp.einsum('bhqk,bhkd->bhqd', a, v)

    o = lax.map(blk, qb)
    return jnp.moveaxis(o, 0, 2).reshape(b, h, tq, v.shape[-1])


def even_mixer(h, hc, w_in, w_out, rpb, ret_decay, with_ctx):
    b, n_tok, _ = h.shape
    qa, ka, va, rq, rk, rv, rg = split_cols(h @ w_in, EVEN_IN_SIZES)
    cqa, cka, cva, crq, crk, crv, crg = split_cols(hc @ w_in, EVEN_IN_SIZES)
    ka_c, va_c = heads(cka, NA_HEADS), heads(cva, NA_HEADS)
    a_l = neighborhood_attention(heads(qa, NA_HEADS), heads(ka, NA_HEADS), heads(va, NA_HEADS), ka_c, va_c, rpb)
    cos, sin = rope_tables(n_tok, RET_DK)
    kscale = RET_DK ** -0.5
    lat = (apply_rope(heads(rq, RET_HEADS), cos, sin), apply_rope(heads(rk, RET_HEADS), cos, sin) * kscale, heads(rv, RET_HEADS))
    cx = (heads(crq, RET_HEADS), heads(crk, RET_HEADS) * kscale, heads(crv, RET_HEADS))
    log_g = -jnp.exp(ret_decay.astype(F32))
    s0 = jnp.zeros((b, RET_HEADS, RET_DK, RET_DV), F32)
    fwd = functools.partial(retention_scan, log_g=log_g[0])
    bwd = functools.partial(retention_scan, log_g=log_g[1])
    r_l, r_c = bidirectional(fwd, bwd, lat, cx, lat, cx, s0, with_ctx)
    ret_l = merge(head_rms(r_l)) * jax.nn.silu(rg)
    y = jnp.concatenate([merge(a_l), ret_l], axis=-1) @ w_out
    yc = None
    if with_ctx:
        a_c = softmax_attention(heads(cqa, NA_HEADS), ka_c, va_c)
        ret_c = merge(head_rms(r_c)) * jax.nn.silu(crg)
        yc = jnp.concatenate([merge(a_c), ret_c], axis=-1) @ w_out
    return y, yc


def odd_mixer(h, hc, w_in, w_out, lam_p, lb, layer_idx, with_ctx):
    b, n_tok, _ = h.shape
    dq, dk, dv, hq, hff, hfb, hi, hg = split_cols(h @ w_in, ODD_IN_SIZES)
    cdq, cdk, cdv, chq, chff, chfb, chi, chg = split_cols(hc @ w_in, ODD_IN_SIZES)
    cos, sin = rope_tables(n_tok, DIFF_DK)
    scale = DIFF_DK ** -0.5
    q_l = apply_rope(diff_heads(dq), cos, sin) * scale
    k_l = apply_rope(diff_heads(dk), cos, sin)
    v_l = heads(dv, DIFF_HEADS)
    k_c, v_c = diff_heads(cdk), heads(cdv, DIFF_HEADS)
    lp = lam_p.astype(F32)
    lam_init = 0.8 - 0.6 * math.exp(-0.3 * layer_idx)
    lam = jnp.exp(jnp.sum(lp[0] * lp[1])) - jnp.exp(jnp.sum(lp[2] * lp[3])) + lam_init
    o_l = diff_attention(q_l, jnp.concatenate([k_l, k_c], axis=3), jnp.concatenate([v_l, v_c], axis=2), lam)
    d_l = merge(head_rms(o_l)) * (1.0 - lam_init)

    def gates(fr):
        f = lb + (1.0 - lb) * jax.nn.sigmoid(fr.astype(F32))
        return heads(jnp.log(f), HG_HEADS), heads(1.0 - f, HG_HEADS)

    def hg_inputs(q_raw, ff_raw, fb_raw, i_raw):
        q = heads(jax.nn.silu(q_raw), HG_HEADS)
        v = heads(i_raw, HG_HEADS)
        lf_f, k_f = gates(ff_raw)
        lf_b, k_b = gates(fb_raw)
        return (q, k_f, v, lf_f), (q, k_b, v, lf_b)

    lat_f, lat_b = hg_inputs(hq, hff, hfb, hi)
    ctx_f, ctx_b = hg_inputs(chq, chff, chfb, chi)
    s0 = jnp.zeros((b, HG_HEADS, HG_DK, HG_DV), F32)
    og_l, og_c = bidirectional(hgrn_scan, hgrn_scan, lat_f, ctx_f, lat_b, ctx_b, s0, with_ctx)
    g_l = merge(head_rms(og_l)) * jax.nn.silu(hg)
    y = jnp.concatenate([d_l, g_l], axis=-1) @ w_out
    yc = None
    if with_ctx:
        o_c = diff_attention(diff_heads(cdq) * scale, k_c, v_c, lam)
        d_c = merge(head_rms(o_c)) * (1.0 - lam_init)
        g_c = merge(head_rms(og_c)) * jax.nn.silu(chg)
        yc = jnp.concatenate([d_c, g_c], axis=-1) @ w_out
    return y, yc


def hier_moe(tok, wg, bg, we, be, w1, w3, w2):
    n, d = tok.shape
    lg_g = (tok @ wg).astype(F32) + bg.astype(F32)
    p_g = jax.nn.softmax(lg_g, axis=-1)
    g_top = jnp.argmax(lg_g, axis=-1)
    w_grp = jnp.take_along_axis(p_g, g_top[:, None], axis=1)
    lg_e = ((tok @ we).astype(F32) + be.astype(F32)).reshape(n, N_GROUPS, EXPERTS_PER_GROUP)
    lg_in = jnp.take_along_axis(lg_e, g_top[:, None, None], axis=1)[:, 0]
    top_v, top_i = lax.top_k(lg_in, TOP_K)
    gates = w_grp * jax.nn.softmax(top_v, axis=-1)
    eid = (g_top[:, None] * EXPERTS_PER_GROUP + top_i).reshape(-1)
    n_slots = n * TOP_K
    order = jnp.argsort(eid)
    s_e = eid[order]
    s_t = (jnp.arange(n_slots) // TOP_K)[order]
    s_w = gates.reshape(-1)[order]
    counts = jnp.zeros((N_EXPERTS,), jnp.int32).at[eid].add(1)
    padded = (counts + MOE_BLOCK - 1) // MOE_BLOCK * MOE_BLOCK
    p_end = jnp.cumsum(padded)
    p_start = p_end - padded
    u_start = jnp.cumsum(counts) - counts
    dest = p_start[s_e] + jnp.arange(n_slots) - u_start[s_e]
    n_rows = -(-n_slots // MOE_BLOCK) * MOE_BLOCK + N_EXPERTS * MOE_BLOCK
    n_blocks = n_rows // MOE_BLOCK
    row_tok = jnp.full((n_rows,), n, jnp.int32).at[dest].set(s_t)
    row_w = jnp.zeros((n_rows,), F32).at[dest].set(s_w)
    blk_e = jnp.minimum(jnp.searchsorted(p_end, jnp.arange(n_blocks) * MOE_BLOCK, side='right'), N_EXPERTS - 1)
    xb = jnp.concatenate([tok, jnp.zeros((1, d), tok.dtype)], axis=0)[row_tok].reshape(n_blocks, MOE_BLOCK, d)

    def expert_block(args):
        xe, e = args
        return (jax.nn.silu(xe @ w1[e]) * (xe @ w3[e])) @ w2[e]

    yb = lax.map(expert_block, (xb, blk_e)).reshape(n_rows, d)
    y = jax.ops.segment_sum(yb * row_w[:, None].astype(yb.dtype), row_tok, num_segments=n + 1)
    return y[:n]


def setup_inputs(seed: int = 0) -> dict:
    key = jax.random.key(seed)
    ks = jax.random.split(key, 24)
    d = D_MODEL

    def nrm(k, shape, scale):
        return jax.random.normal(k, shape, F32) * scale

    ret_base = jnp.log(-jnp.log1p(-jnp.exp2(-5.0 - jnp.arange(RET_HEADS, dtype=F32))))
    return {
        'x': nrm(ks[0], (BATCH, SEQ, d), 1.0),
        'c': nrm(ks[1], (BATCH, d), 1.0),
        'ctx': nrm(ks[2], (BATCH, CTX_LEN, d), 1.0),
        'c_ctx': nrm(ks[3], (d,), 1.0),
        'ada_w': nrm(ks[4], (DEPTH, d, 6 * d), 0.5 * d ** -0.5),
        'ada_b': nrm(ks[5], (DEPTH, 6 * d), 0.02),
        'norm1_w': 1.0 + nrm(ks[6], (DEPTH, d), 0.02),
        'norm2_w': 1.0 + nrm(ks[7], (DEPTH, d), 0.02),
        'w_in_even': nrm(ks[8], (N_EVEN, d, sum(EVEN_IN_SIZES)), d ** -0.5),
        'w_out_even': nrm(ks[9], (N_EVEN, EVEN_MIX, d), EVEN_MIX ** -0.5),
        'na_rpb': nrm(ks[10], (N_EVEN, NA_HEADS, 2 * NA_WIN_H - 1, 2 * NA_WIN_W - 1), 0.1),
        'ret_decay': ret_base + nrm(ks[11], (N_EVEN, 2, RET_HEADS), 0.05),
        'w_in_odd': nrm(ks[12], (N_ODD, d, sum(ODD_IN_SIZES)), d ** -0.5),
        'w_out_odd': nrm(ks[13], (N_ODD, ODD_MIX, d), ODD_MIX ** -0.5),
        'diff_lambda': nrm(ks[14], (N_ODD, 4, DIFF_DK), 0.1),
        'hg_lb_logits': nrm(ks[15], (DEPTH, HG_HEADS * HG_DK), 0.5),
        'router_g_w': nrm(ks[16], (DEPTH, d, N_GROUPS), d ** -0.5),
        'router_g_b': nrm(ks[17], (DEPTH, N_GROUPS), 0.01),
        'router_e_w': nrm(ks[18], (DEPTH, d, N_EXPERTS), d ** -0.5),
        'router_e_b': nrm(ks[19], (DEPTH, N_EXPERTS), 0.01),
        'moe_w1': nrm(ks[20], (DEPTH, N_EXPERTS, d, EXPERT_FF), d ** -0.5),
        'moe_w3': nrm(ks[21], (DEPTH, N_EXPERTS, d, EXPERT_FF), d ** -0.5),
        'moe_w2': nrm(ks[22], (DEPTH, N_EXPERTS, EXPERT_FF, d), EXPERT_FF ** -0.5),
        'norm_f_w': 1.0 + nrm(ks[23], (d,), 0.02),
    }


def reference(x, c, ctx, c_ctx, ada_w, ada_b, norm1_w, norm2_w, w_in_even, w_out_even, na_rpb, ret_decay,
              w_in_odd, w_out_odd, diff_lambda, hg_lb_logits, router_g_w, router_g_b, router_e_w, router_e_b,
              moe_w1, moe_w3, moe_w2, norm_f_w):
    b, n_tok, d = x.shape
    n_ctx = ctx.shape[1]
    lb_all = jnp.cumsum(jax.nn.softmax(hg_lb_logits.astype(F32), axis=0), axis=0)
    lb_all = lb_all - lb_all[0]
    sc = jax.nn.silu(c)
    scc = jax.nn.silu(c_ctx)
    for l in range(DEPTH):
        with_ctx = l < DEPTH - 1
        mod = (sc @ ada_w[l] + ada_b[l])[:, None, :]
        mod_c = (scc @ ada_w[l] + ada_b[l])[None, None, :]
        sh1, s1, g1, sh2, s2, g2 = jnp.split(mod, 6, axis=-1)
        csh1, cs1, cg1, csh2, cs2, cg2 = jnp.split(mod_c, 6, axis=-1)
        h = rms_norm(x, norm1_w[l]) * (1.0 + s1) + sh1
        hc = rms_norm(ctx, norm1_w[l]) * (1.0 + cs1) + csh1
        j = l // 2
        if l % 2 == 0:
            y, yc = even_mixer(h, hc, w_in_even[j], w_out_even[j], na_rpb[j], ret_decay[j], with_ctx)
        else:
            y, yc = odd_mixer(h, hc, w_in_odd[j], w_out_odd[j], diff_lambda[j], lb_all[l], l, with_ctx)
        x = x + g1 * y
        h2 = rms_norm(x, norm2_w[l]) * (1.0 + s2) + sh2
        moe_args = (router_g_w[l], router_g_b[l], router_e_w[l], router_e_b[l], moe_w1[l], moe_w3[l], moe_w2[l])
        if with_ctx:
            ctx = ctx + cg1 * yc
            hc2 = rms_norm(ctx, norm2_w[l]) * (1.0 + cs2) + csh2
            tok = jnp.concatenate([h2.reshape(b * n_tok, d), hc2.reshape(b * n_ctx, d)], axis=0)
            out = hier_moe(tok, *moe_args)
            x = x + g2 * out[:b * n_tok].reshape(b, n_tok, d)
            ctx = ctx + cg2 * out[b * n_tok:].reshape(b, n_ctx, d)
        else:
            x = x + g2 * hier_moe(h2.reshape(b * n_tok, d), *moe_args).reshape(b, n_tok, d)
    return rms_norm(x, norm_f_w)
```

```python
import numpy as np
import concourse.bass as bass
import concourse.mybir as mybir
from concourse.bass_utils import run_bass_kernel_spmd
from contextlib import ExitStack

F32 = mybir.dt.float32
BF16 = mybir.dt.bfloat16
AF = mybir.ActivationFunctionType
ALU = mybir.AluOpType
AX = mybir.AxisListType
DT_SIZE = {F32: 4, BF16: 2, mybir.dt.int32: 4, mybir.dt.uint32: 4}

SEM_LIM = 30000
DMA_SLOTS = 6


class Sched:
    ENGS = ("pe", "act", "dve", "pool", "sp")

    def __init__(self, nc, stack):
        self.nc = nc
        self.stack = stack
        self.ops = {e: [] for e in self.ENGS}
        self.cnt = {e: 0 for e in self.ENGS}
        self.sems = {}
        self.acc = {}
        self.dma_slots = {}
        self.dma_i = {q: 0 for q in self.ENGS}
        self.nsem = 0
        self.out_sigs = []

    def _newsem(self):
        self.nsem += 1
        return self.stack.enter_context(self.nc.semaphore("s%d" % self.nsem))

    def _sig_compute(self, eng):
        k = self.cnt[eng]
        self.cnt[eng] += 1
        seg, val = k // SEM_LIM, k % SEM_LIM + 1
        if (eng, seg) not in self.sems:
            self.sems[(eng, seg)] = self._newsem()
        return (self.sems[(eng, seg)], val)

    @staticmethod
    def _interval(ap):
        t = ap.tensor
        name = t.name
        esz = DT_SIZE[ap.dtype]
        dims = ap.ap
        off = ap.offset
        space = str(ap.space)
        if space in ("SB", "PSUM"):
            pstep = dims[0][0]
            lo = off % pstep if pstep > 0 else off
            rest = dims[1:]
        else:
            lo = off
            rest = dims
        span = 0
        for st, n in rest:
            span += abs(st) * (n - 1)
        return name, lo * esz, (lo + span + 1) * esz

    def _deps(self, reads, writes, sig, eng):
        deps = []
        for ap, is_w in [(a, False) for a in reads] + [(a, True) for a in writes]:
            name, lo, hi = self._interval(ap)
            lst = self.acc.setdefault(name, [])
            keep = []
            for ent in lst:
                elo, ehi, esig, ew, eeng = ent
                if esig is sig:
                    keep.append(ent)
                    continue
                ov = elo < hi and lo < ehi
                if ov and (ew or is_w):
                    deps.append((esig, eeng))
                contained = lo <= elo and ehi <= hi
                if contained and (is_w or (not ew and eeng == eng and eng != "dma")):
                    continue
                keep.append(ent)
            keep.append([lo, hi, sig, is_w, eng])
            self.acc[name] = keep
        return deps

    def op(self, eng, fn, reads, writes):
        sig = self._sig_compute(eng)
        deps = self._deps(reads, writes, sig, eng)
        self.ops[eng].append((fn, sig, deps, False))
        return sig

    def dma(self, out, in_, q="sp", is_output=False, **kw):
        slots = self.dma_slots.setdefault(q, [[None, 0] for _ in range(DMA_SLOTS)])
        i = self.dma_i[q]
        self.dma_i[q] += 1
        slot = slots[i % DMA_SLOTS]
        deps = []
        if slot[0] is None or slot[1] >= SEM_LIM // 16:
            if slot[0] is not None:
                deps.append(((slot[0], slot[1] * 16), "dma"))
            slot[0] = self._newsem()
            slot[1] = 0
        if slot[1] > 0:
            deps.append(((slot[0], slot[1] * 16), "dma"))
        slot[1] += 1
        sig = (slot[0], slot[1] * 16)
        deps += self._deps([in_], [out], sig, "dma")
        fn = lambda e, out=out, in_=in_, kw=kw: e.dma_start(out=out, in_=in_, **kw)
        self.ops[q].append((fn, sig, deps, True))
        if is_output:
            self.out_sigs.append(sig)
        return sig

    def emit(self):
        nc = self.nc
        final_waits = []
        for q, slots in self.dma_slots.items():
            for s, c in slots:
                if s is not None and c > 0:
                    final_waits.append((s, c * 16))
        with nc.Block() as block:
            def run(eng_name, e):
                known = {}
                for fn, sig, deps, is_dma in self.ops[eng_name]:
                    for (dsem, dval), deng in deps:
                        if deng == "pe" and eng_name == "pe" and not is_dma:
                            continue
                        key = id(dsem)
                        if known.get(key, 0) >= dval:
                            continue
                        e.wait_ge(dsem, dval)
                        known[key] = dval
                    ins = fn(e)
                    ins.then_inc(sig[0], 16 if is_dma else 1)
                if eng_name == "sp":
                    for s, v in final_waits:
                        e.wait_ge(s, v)

            @block.tensor
            def _(e):
                run("pe", e)

            @block.scalar
            def _(e):
                run("act", e)

            @block.vector
            def _(e):
                run("dve", e)

            @block.gpsimd
            def _(e):
                run("pool", e)

            @block.sync
            def _(e):
                run("sp", e)

    def mm(self, out, lhsT, rhs, start=True, stop=True):
        return self.op("pe", lambda e: e.matmul(out, lhsT, rhs, start=start, stop=stop),
                       [lhsT, rhs], [out])

    def transpose(self, out, in_, ident):
        return self.op("pe", lambda e: e.transpose(out, in_, ident), [in_, ident], [out])

    def act(self, out, in_, func, bias=None, scale=None, accum_out=None, eng="act"):
        reads = [in_]
        kw = {}
        if bias is not None:
            kw["bias"] = bias
            if not isinstance(bias, (int, float)):
                reads.append(bias)
        if scale is not None:
            kw["scale"] = scale
            if not isinstance(scale, (int, float)):
                reads.append(scale)
        writes = [out]
        if accum_out is not None:
            kw["accum_out"] = accum_out
            writes.append(accum_out)
        return self.op(eng, lambda e: e.activation(out=out, in_=in_, func=func, **kw), reads, writes)

    def tt(self, out, in0, in1, op, eng="dve"):
        return self.op(eng, lambda e: e.tensor_tensor(out, in0, in1, op), [in0, in1], [out])

    def ts(self, out, in0, s1, s2=None, op0=ALU.mult, op1=None, eng="dve", accum_out=None):
        reads = [in0]
        for s in (s1, s2):
            if s is not None and not isinstance(s, (int, float)):
                reads.append(s)
        writes = [out]
        kw = {}
        if op1 is not None:
            kw["op1"] = op1
        if accum_out is not None:
            kw["accum_out"] = accum_out
            writes.append(accum_out)
        return self.op(eng, lambda e: e.tensor_scalar(out, in0, s1, s2, op0, **kw), reads, writes)

    def stt(self, out, in0, scalar, in1, op0, op1, eng="dve"):
        reads = [in0, in1]
        if not isinstance(scalar, (int, float)):
            reads.append(scalar)
        return self.op(eng, lambda e: e.scalar_tensor_tensor(out, in0, scalar, in1, op0, op1), reads, [out])

    def copy(self, out, in_, eng="dve"):
        if eng == "act":
            return self.act(out, in_, AF.Copy)
        return self.op(eng, lambda e: e.tensor_copy(out, in_), [in_], [out])

    def memset(self, ap, val, eng="dve"):
        return self.op(eng, lambda e: e.memset(ap, val), [], [ap])

    def reduce(self, out, in_, op=ALU.add, axis=AX.X, eng="dve"):
        return self.op(eng, lambda e: e.tensor_reduce(out, in_, axis, op), [in_], [out])

    def recip(self, out, in_):
        return self.op("dve", lambda e: e.reciprocal(out, in_), [in_], [out])

    def scan(self, out, d0, d1, initial, op0, op1):
        reads = [d0, d1]
        if not isinstance(initial, (int, float)):
            reads.append(initial)
        return self.op("dve", lambda e: e.tensor_tensor_scan(out, d0, d1, initial, op0, op1), reads, [out])

    def max8(self, out, in_):
        return self.op("dve", lambda e: e.max(out, in_), [in_], [out])

D = 2048
NT = 2304
NLAT = 2048
NCTX = 256
EPS = 1e-6
NEG = -30000.0
ARENA = 47 * 1024

LL_OFF = 1920
LL_U = 3968
LC_OFF = 128
LC_U = 2176


def _rope_tables(dim, nheadrep):
    n_f = dim // 4
    inv = (10000.0 ** (-np.arange(n_f, dtype=np.float32) / n_f)).astype(np.float32)
    t = np.arange(NLAT)
    pos = np.stack([t // 64, t % 64], axis=-1).astype(np.float32)
    ang = (pos[:, :, None] * inv).astype(np.float32)
    cos = np.cos(ang).astype(np.float32)
    sin = np.sin(ang).astype(np.float32)
    C = np.zeros((128, NLAT), np.float32)
    S = np.zeros((128, NLAT), np.float32)
    PM = np.zeros((128, 128), np.float32)
    for p in range(128):
        dd = p % dim
        axis = dd // (dim // 2)
        half = (dd % (dim // 2)) // n_f
        f = dd % n_f
        C[p] = cos[:, axis, f]
        S[p] = sin[:, axis, f] * (-1.0 if half == 0 else 1.0)
        partner = p + n_f if half == 0 else p - n_f
        PM[partner, p] = 1.0
    return C, S, PM


def _na_bias(rpb):
    out = np.full((8, 7, 128, 7, 128), NEG, np.float32)
    type_i = [0, 1, 2, 3, 13, 14, 15]
    b = np.arange(128)[:, None]
    n = np.arange(128)[None, :]
    for ti, i in enumerate(type_i):
        t = 128 * i + n
        r = t // 64
        c = t % 64
        rs = np.clip(r - 4, 0, 24)
        cs = np.clip(c - 8, 0, 48)
        for jj in range(7):
            j = i - 3 + jj
            if j < 0 or j > 15:
                continue
            s = 128 * j + b
            kr = s // 64
            kc = s % 64
            ok = (kr >= rs) & (kr < rs + 8) & (kc >= cs) & (kc < cs + 16)
            ro = np.clip(kr - r + 7, 0, 14)
            co = np.clip(kc - c + 15, 0, 30)
            ro, co, ok = np.broadcast_arrays(ro, co, ok)
            for h in range(8):
                vals = rpb[h][ro, co]
                out[h, ti, :, jj, :] = np.where(ok, vals, np.float32(NEG))
    return out


def _na_type(i):
    if i < 3:
        return i
    if i <= 12:
        return 3
    return i - 9


def _const_tables():
    C = {}
    C["ones"] = np.ones((128, 128), np.float32)
    C["ident"] = np.eye(128, dtype=np.float32)
    c1, s1, p1 = _rope_tables(128, 1)
    c2, s2, p2 = _rope_tables(64, 2)
    C["cos128"], C["sin128"], C["perm128"] = c1, s1, p1
    C["cos64"], C["sin64"], C["perm64"] = c2, s2, p2
    b = np.arange(128, dtype=np.float64)[:, None]
    u = np.arange(LL_U, dtype=np.float64)[None, :]
    E = u - b - LL_OFF
    BIG = 1.0e7
    C["epm"] = np.where(E >= 0, E, BIG).astype(np.float32)
    C["enm"] = np.where(E <= 0, -E, BIG).astype(np.float32)
    u2 = np.arange(LC_U, dtype=np.float64)[None, :]
    dl = u2 - b - LC_OFF
    C["ec1"] = (dl + 256.0).astype(np.float32)
    C["ec2"] = (2048.0 - dl).astype(np.float32)
    n = np.arange(128)[None, :]
    bb = np.arange(128)[:, None]
    C["trilT"] = (bb <= n).astype(np.float32)
    C["triuT"] = (bb >= n).astype(np.float32)
    return C


def host_prep(inputs):
    f32 = np.float32
    g = {k: np.ascontiguousarray(np.asarray(v, dtype=f32)) for k, v in inputs.items()}
    shared = {}
    shared["ada_w"] = g["ada_w"]
    shared["ada_b"] = np.ascontiguousarray(g["ada_b"].reshape(2, 96, 128).transpose(0, 2, 1))
    shared["n1w"] = np.ascontiguousarray(g["norm1_w"].reshape(2, 16, 128).transpose(0, 2, 1))
    shared["n2w"] = np.ascontiguousarray(g["norm2_w"].reshape(2, 16, 128).transpose(0, 2, 1))
    shared["nfw"] = np.ascontiguousarray(g["norm_f_w"].reshape(16, 128).T)
    shared["w_in_even"] = g["w_in_even"][0]
    shared["w_out_even"] = g["w_out_even"][0]
    shared["w_in_odd"] = g["w_in_odd"][0]
    shared["w_out_odd"] = g["w_out_odd"][0]
    shared["nabias"] = _na_bias(g["na_rpb"][0])
    shared["retdec"] = np.ascontiguousarray(np.broadcast_to(g["ret_decay"][0].reshape(1, 16), (128, 16)))
    shared["dlam"] = np.ascontiguousarray(np.broadcast_to(g["diff_lambda"][0].reshape(1, 4, 64), (128, 4, 64)))
    shared["hglb"] = np.ascontiguousarray(g["hg_lb_logits"].reshape(2, 8, 128).transpose(2, 0, 1))
    wr = np.concatenate([g["router_g_w"], g["router_e_w"]], axis=-1)
    shared["wr"] = np.ascontiguousarray(wr.reshape(2, 16, 128, 36).transpose(0, 2, 1, 3))
    br = np.concatenate([g["router_g_b"], g["router_e_b"]], axis=-1)
    shared["br"] = np.ascontiguousarray(np.broadcast_to(br[:, None, :], (2, 128, 36)))
    shared["moe_w1"] = g["moe_w1"]
    shared["moe_w3"] = g["moe_w3"]
    shared["moe_w2"] = g["moe_w2"]
    shared.update(_const_tables())
    per_core = []
    for bi in range(g["x"].shape[0]):
        xin = np.concatenate([g["x"][bi].T, g["ctx"][bi].T], axis=1)
        cT = np.stack([g["c"][bi].reshape(16, 128).T, g["c_ctx"].reshape(16, 128).T], axis=-1)
        m = dict(shared)
        m["xin"] = np.ascontiguousarray(xin.reshape(16, 128, NT))
        m["cT"] = np.ascontiguousarray(cT)
        per_core.append(m)
    return per_core


INPUT_SHAPES = {
    "xin": [16, 128, NT], "cT": [128, 16, 2], "ada_w": [2, D, 6 * D], "ada_b": [2, 128, 96],
    "n1w": [2, 128, 16], "n2w": [2, 128, 16], "nfw": [128, 16],
    "w_in_even": [D, 9216], "w_out_even": [3072, D], "w_in_odd": [D, 8192], "w_out_odd": [D, D],
    "nabias": [8, 7, 128, 7, 128], "retdec": [128, 16], "dlam": [128, 4, 64], "hglb": [128, 2, 8],
    "wr": [2, 128, 16, 36], "br": [2, 128, 36],
    "moe_w1": [2, 32, D, 1024], "moe_w3": [2, 32, D, 1024], "moe_w2": [2, 32, 1024, D],
    "ones": [128, 128], "ident": [128, 128], "cos128": [128, NLAT], "sin128": [128, NLAT], "perm128": [128, 128],
    "cos64": [128, NLAT], "sin64": [128, NLAT], "perm64": [128, 128],
    "epm": [128, LL_U], "enm": [128, LL_U], "ec1": [128, LC_U], "ec2": [128, LC_U],
    "trilT": [128, 128], "triuT": [128, 128],
}


class Arena:
    def __init__(self, ap, size):
        self.ap = ap
        self.size = size
        self.off = 0

    def alloc(self, *free):
        n = 1
        for f in free:
            n *= f
        assert self.off + n <= self.size, ("arena overflow", self.off, n, self.size)
        v = self.ap[:, self.off:self.off + n]
        self.off += n
        if len(free) == 2:
            v = v.rearrange("p (a b) -> p a b", a=free[0])
        elif len(free) == 3:
            v = v.rearrange("p (a b c) -> p a b c", a=free[0], b=free[1])
        return v

    def mark(self):
        return self.off

    def release(self, m):
        self.off = m


TOKBLOCKS = [(0, 512, 0), (512, 512, 0), (1024, 512, 0), (1536, 512, 0), (2048, 256, 1)]


class Rot:
    def __init__(self, items):
        self.items = list(items)
        self.i = 0

    def __call__(self):
        x = self.items[self.i % len(self.items)]
        self.i += 1
        return x


def build_program(dbg=None, stages=None, use_bf16_moe=False, tiny=()):
    dbg = dbg or ()
    nc = bass.Bass("TRN2", target_bir_lowering=False)
    stack = ExitStack()
    I = {k: nc.dram_tensor(k, ([1] * len(shp) if k in tiny else shp), F32, kind="ExternalInput").ap() for k, shp in INPUT_SHAPES.items()}
    outT = nc.dram_tensor("outT", [16, 128, NLAT], F32, kind="ExternalOutput").ap()

    def scratch(name, shape):
        kind = "ExternalOutput" if name in dbg else "Internal"
        return nc.dram_tensor(name, shape, F32, kind=kind).ap()

    XT = scratch("XT", [16, 128, NT])
    H2T = scratch("H2T", [16, 128, NT])
    FM = scratch("FM", [64, 128, NT])
    TM = scratch("TM", [NT, 3072])
    MIX = scratch("MIX", [24, 128, NT])
    DBG = scratch("DBG", [128, 4096])

    SBt = stack.enter_context(nc.sbuf_tensor("SB", [128, ARENA], F32))
    PS = [stack.enter_context(nc.psum_tensor("ps%d" % i, [128, 512], F32))[:, :] for i in range(8)]
    S = Sched(nc, stack)
    A = Arena(SBt, ARENA)

    ones = A.alloc(128)
    ident = A.alloc(128)
    sc = A.alloc(16, 2)
    MOD = [A.alloc(96, 2) for _ in range(2)]
    AN = [A.alloc(2, 16, 2) for _ in range(2)]
    NW = [[A.alloc(16) for _ in range(2)] for _ in range(2)]
    NFW = A.alloc(16)
    GATES = A.alloc(18, 32)
    epsc = A.alloc(1)
    S.memset(epsc, EPS)
    S.dma(ones, I["ones"])
    S.dma(ident, I["ident"])
    S.dma(sc, I["cT"])
    S.dma(NFW, I["nfw"])
    for l in range(2):
        S.dma(NW[l][0], I["n1w"][l])
        S.dma(NW[l][1], I["n2w"][l])
    S.act(sc, sc, AF.Silu)

    def mod_ap(l, m, j, v):
        return MOD[l][:, m * 16 + j, v:v + 1]

    def adaln(l):
        mk = A.mark()
        adab = A.alloc(96)
        S.dma(adab, I["ada_b"][l])
        wbuf = Rot([A.alloc(16, 128) for _ in range(3)])
        pr = Rot(PS[0:4])
        for j in range(96):
            w = wbuf()
            S.dma(w, I["ada_w"][l, :, j * 128:(j + 1) * 128].rearrange("(kc p) n -> p kc n", p=128))
            ps = pr()
            for kc in range(16):
                S.mm(ps[:, 0:2], w[:, kc, :], sc[:, kc, :], start=(kc == 0), stop=(kc == 15))
            S.act(MOD[l][:, j, :], ps[:, 0:2], AF.Identity, bias=adab[:, j:j + 1])
        for ni, m in ((0, 1), (1, 4)):
            for v in range(2):
                S.stt(AN[l][:, ni, :, v], MOD[l][:, m * 16:(m + 1) * 16, v], 1.0, NW[l][ni], ALU.add, ALU.mult)
        A.release(mk)

    def rms_mod(xblk, n, scale_ap, bias_ap, out, tmp2, rstd, psb):
        for j in range(16):
            t = tmp2()
            S.act(t[:, :n], xblk[:, j, :n], AF.Square)
            S.mm(psb[:, :n], ones, t[:, :n], start=(j == 0), stop=(j == 15))
        S.act(rstd[:, :n], psb[:, :n], AF.Ln, scale=1.0 / 2048, bias=epsc)
        S.act(rstd[:, :n], rstd[:, :n], AF.Exp, scale=-0.5)
        for j in range(16):
            t = tmp2()
            S.tt(t[:, :n], xblk[:, j, :n], rstd[:, :n], ALU.mult)
            b = bias_ap(j) if bias_ap is not None else 0.0
            S.act(out[:, j, :n], t[:, :n], AF.Identity, scale=scale_ap(j), bias=b)

    def inproj(l, src, w_in, fm_specs, tm_specs, rope):
        mk = A.mark()
        xblk = A.alloc(16, 512)
        hT = A.alloc(16, 512)
        tmp2 = Rot([A.alloc(512) for _ in range(2)])
        rstd = A.alloc(512)
        wfm = Rot([A.alloc(16, 128) for _ in range(3)])
        wtm = Rot([A.alloc(16, 256) for _ in range(2)])
        obuf = Rot([A.alloc(512) for _ in range(4)])
        tbuf = Rot([A.alloc(512) for _ in range(4)])
        otm = Rot([A.alloc(256) for _ in range(3)])
        if rope is not None:
            cosT = A.alloc(NLAT)
            sinT = A.alloc(NLAT)
            perm = A.alloc(128)
            S.dma(cosT, I[rope[0]])
            S.dma(sinT, I[rope[1]])
            S.dma(perm, I[rope[2]])
        if l == 1:
            lbt = A.alloc(2, 8)
            lb = A.alloc(8)
            oml = A.alloc(8)
            S.dma(lbt, I["hglb"])
            S.tt(lb, lbt[:, 1, :], lbt[:, 0, :], ALU.subtract)
            S.act(lb, lb, AF.Sigmoid)
            S.ts(oml, lb, -1.0, 1.0, ALU.mult, ALU.add)
        pfm = Rot(PS[1:5])
        pperm = Rot(PS[5:7])
        ptm = Rot(PS[5:8])
        for (t0, n, v) in TOKBLOCKS:
            S.dma(xblk[:, :, :n], src[:, :, t0:t0 + n].rearrange("c p t -> p c t"))
            rms_mod(xblk, n, lambda j: AN[l][:, 0, j, v:v + 1], lambda j: mod_ap(l, 0, j, v), hT, tmp2, rstd, PS[0])
            for (col0, nch, dst0, kind, arg) in fm_specs:
                for ci in range(nch):
                    w = wfm()
                    c0 = col0 + ci * 128
                    S.dma(w, w_in[:, c0:c0 + 128].rearrange("(kc p) n -> p kc n", p=128))
                    ps = pfm()
                    for kc in range(16):
                        S.mm(ps[:, :n], w[:, kc, :], hT[:, kc, :n], start=(kc == 0), stop=(kc == 15))
                    o = obuf()
                    if kind == "scale":
                        S.act(o[:, :n], ps[:, :n], AF.Copy, scale=arg)
                    elif kind == "silu":
                        S.act(o[:, :n], ps[:, :n], AF.Silu)
                    elif kind == "rope":
                        if v == 1:
                            S.act(o[:, :n], ps[:, :n], AF.Copy, scale=arg)
                        else:
                            xs = tbuf()
                            S.act(xs[:, :n], ps[:, :n], AF.Copy, scale=arg)
                            p2 = pperm()
                            S.mm(p2[:, :n], perm, xs[:, :n])
                            t2 = tbuf()
                            S.tt(t2[:, :n], p2[:, :n], sinT[:, t0:t0 + n], ALU.mult)
                            S.tt(xs[:, :n], xs[:, :n], cosT[:, t0:t0 + n], ALU.mult, eng="pool")
                            S.tt(o[:, :n], xs[:, :n], t2[:, :n], ALU.add)
                    elif kind == "gate":
                        f = tbuf()
                        S.act(f[:, :n], ps[:, :n], AF.Sigmoid)
                        S.ts(f[:, :n], f[:, :n], oml[:, ci:ci + 1], lb[:, ci:ci + 1], ALU.mult, ALU.add)
                        S.ts(o[:, :n], f[:, :n], -1.0, 1.0, ALU.mult, ALU.add)
                        o2 = obuf()
                        S.act(o2[:, :n], f[:, :n], AF.Ln)
                        S.dma(FM[arg + ci, :, t0:t0 + n], o2[:, :n], q="pool")
                    S.dma(FM[dst0 + ci, :, t0:t0 + n], o[:, :n], q="pool")
            for (col0, ncols, tc0) in tm_specs:
                for pc in range(ncols // 256):
                    w = wtm()
                    c0 = col0 + pc * 256
                    S.dma(w, w_in[:, c0:c0 + 256].rearrange("(kc p) n -> p kc n", p=128))
                    for tt_ in range(n // 128):
                        ps = ptm()
                        for kc in range(16):
                            S.mm(ps[:, :256], hT[:, kc, tt_ * 128:(tt_ + 1) * 128], w[:, kc, :],
                                 start=(kc == 0), stop=(kc == 15))
                        o = otm()
                        S.copy(o, ps[:, :256], eng="act" if tt_ % 2 else "dve")
                        r0 = t0 + tt_ * 128
                        S.dma(TM[r0:r0 + 128, tc0 + pc * 256: tc0 + (pc + 1) * 256], o, q="pool")
        A.release(mk)

    def na_stage():
        mk = A.mark()
        qT = A.alloc(NT)
        kT = A.alloc(NT)
        V = A.alloc(18, 128)
        OUT = A.alloc(NT)
        bias = Rot([A.alloc(7, 128) for _ in range(2)])
        Pb = Rot([A.alloc(9, 128) for _ in range(2)])
        rz = Rot([A.alloc(256) for _ in range(2)])
        psS = Rot([(PS[0], PS[1], PS[2]), (PS[3], PS[4], PS[5])])
        for h in range(8):
            S.dma(qT, FM[h])
            S.dma(kT, FM[8 + h])
            S.dma(V, TM[:, h * 128:(h + 1) * 128].rearrange("(j p) d -> p j d", p=128))
            for i in range(16):
                jl = [j for j in range(i - 3, i + 4) if 0 <= j <= 15]
                jj0 = jl[0] - (i - 3)
                bt = bias()
                S.dma(bt, I["nabias"][h, _na_type(i)])
                keys = jl + [16, 17]
                nk = len(keys)
                banks = psS()
                P = Pb()

                def sc_ap(idx):
                    return banks[idx // 4][:, (idx % 4) * 128:(idx % 4 + 1) * 128]
                for idx, j in enumerate(keys):
                    S.mm(sc_ap(idx), kT[:, j * 128:(j + 1) * 128], qT[:, i * 128:(i + 1) * 128])
                nl = len(jl)
                idx = 0
                while idx < nl:
                    g = min(4 - idx % 4, nl - idx)
                    bk = banks[idx // 4]
                    c0 = (idx % 4) * 128
                    S.tt(P[:, idx:idx + g, :], bk[:, c0:c0 + g * 128].rearrange("p (a b) -> p a b", a=g),
                         bt[:, jj0 + idx:jj0 + idx + g, :], ALU.add)
                    idx += g
                S.act(P[:, 0:nl, :], P[:, 0:nl, :], AF.Exp)
                for idx in range(nl, nk):
                    S.act(P[:, idx, :], sc_ap(idx), AF.Exp)
                po = PS[6]
                pz = PS[7]
                for idx, j in enumerate(keys):
                    S.mm(po[:, 0:128], V[:, j, :], P[:, idx, :], start=(idx == 0), stop=(idx == nk - 1))
                for idx, j in enumerate(keys):
                    S.mm(pz[:, 0:128], ones, P[:, idx, :], start=(idx == 0), stop=(idx == nk - 1))
                r = rz()
                S.recip(r[:, 0:128], pz[:, 0:128])
                S.tt(OUT[:, i * 128:(i + 1) * 128], po[:, 0:128], r[:, 0:128], ALU.mult)
            banks = psS()
            P = Pb()
            Pf = P.rearrange("p a b -> p (a b)")
            for idx, j in enumerate((16, 17)):
                S.mm(banks[idx][:, 0:256], kT[:, j * 128:(j + 1) * 128], qT[:, NLAT:NT])
                S.act(Pf[:, 256 * idx:256 * idx + 256], banks[idx][:, 0:256], AF.Exp)
            po = PS[6]
            pz = PS[7]
            for idx, j in enumerate((16, 17)):
                pv = Pf[:, 256 * idx:256 * idx + 256]
                S.mm(po[:, 0:256], V[:, j, :], pv, start=(idx == 0), stop=(idx == 1))
            for idx, j in enumerate((16, 17)):
                pv = Pf[:, 256 * idx:256 * idx + 256]
                S.mm(pz[:, 0:256], ones, pv, start=(idx == 0), stop=(idx == 1))
            r = rz()
            S.recip(r, pz[:, 0:256])
            S.tt(OUT[:, NLAT:NT], po[:, 0:256], r, ALU.mult)
            S.dma(MIX[h], OUT, q="pool")
        A.release(mk)

    def ret_stage():
        mk = A.mark()
        qT = A.alloc(NT)
        kT = A.alloc(NT)
        V = A.alloc(18, 256)
        MLL = A.alloc(LL_U)
        MLC = A.alloc(LC_U)
        tabA = A.alloc(LL_U)
        tabB = A.alloc(LL_U)
        lg = A.alloc(16)
        RG = A.alloc(2, 512)
        Pb = Rot([A.alloc(512) for _ in range(3)])
        sq = Rot([A.alloc(512) for _ in range(2)])
        rstd = A.alloc(512)
        ob = Rot([A.alloc(512) for _ in range(3)])
        S.dma(lg, I["retdec"])
        S.act(lg, lg, AF.Exp)
        S.ts(lg, lg, -1.0, None, ALU.mult)
        pss = Rot(PS[0:4])
        for h in range(8):
            S.dma(qT, FM[16 + h])
            S.dma(kT, FM[24 + h])
            S.dma(V, TM[:, 1024 + h * 256:1024 + (h + 1) * 256].rearrange("(j p) d -> p j d", p=128))
            lgf = lg[:, h:h + 1]
            lgb = lg[:, 8 + h:9 + h]
            S.dma(tabA, I["epm"])
            S.dma(tabB, I["enm"])
            S.act(MLL, tabA, AF.Exp, scale=lgf)
            S.act(tabB, tabB, AF.Exp, scale=lgb)
            S.tt(MLL, MLL, tabB, ALU.add)
            S.dma(tabA[:, 0:LC_U], I["ec1"])
            S.dma(tabB[:, 0:LC_U], I["ec2"])
            S.act(MLC, tabA[:, 0:LC_U], AF.Exp, scale=lgf)
            S.act(tabB[:, 0:LC_U], tabB[:, 0:LC_U], AF.Exp, scale=lgb)
            S.tt(MLC, MLC, tabB[:, 0:LC_U], ALU.add)
            for (t0, n, v) in TOKBLOCKS:
                qb = t0 // 512
                keys = list(range(18)) if v == 0 else [16, 17]
                acc = (PS[4], PS[5])
                for idx, j in enumerate(keys):
                    ps = pss()
                    S.mm(ps[:, :n], kT[:, j * 128:(j + 1) * 128], qT[:, t0:t0 + n])
                    if v == 0 and j < 16:
                        u0 = 512 * qb - 128 * j + LL_OFF
                        msk = MLL[:, u0:u0 + n]
                    elif v == 0:
                        u0 = 512 * qb - 128 * (j - 16) + LC_OFF
                        msk = MLC[:, u0:u0 + n]
                    else:
                        u0 = -128 * (j - 16) + LL_OFF
                        msk = MLL[:, u0:u0 + n]
                    P = Pb()
                    S.tt(P[:, :n], ps[:, :n], msk, ALU.mult)
                    for c in range(2):
                        S.mm(acc[c][:, :n], V[:, j, c * 128:(c + 1) * 128], P[:, :n],
                             start=(idx == 0), stop=(idx == len(keys) - 1))
                S.dma(RG[:, :, :n], FM[32 + 2 * h:34 + 2 * h, :, t0:t0 + n].rearrange("c p t -> p c t"))
                pm = PS[6]
                for c in range(2):
                    s_ = sq()
                    S.act(s_[:, :n], acc[c][:, :n], AF.Square)
                    S.mm(pm[:, :n], ones, s_[:, :n], start=(c == 0), stop=(c == 1))
                S.act(rstd[:, :n], pm[:, :n], AF.Ln, scale=1.0 / 256, bias=epsc)
                S.act(rstd[:, :n], rstd[:, :n], AF.Exp, scale=-0.5)
                for c in range(2):
                    o = ob()
                    S.tt(o[:, :n], acc[c][:, :n], rstd[:, :n], ALU.mult)
                    S.tt(o[:, :n], o[:, :n], RG[:, c, :n], ALU.mult, eng="pool")
                    S.dma(MIX[8 + 2 * h + c, :, t0:t0 + n], o[:, :n], q="pool")
        A.release(mk)

    def post_stage(l, src, w_out, nmix, blocks):
        mk = A.mark()
        mixb = A.alloc(nmix, 512)
        xblk = A.alloc(16, 512)
        h2 = A.alloc(16, 512)
        wb = Rot([A.alloc(nmix, 128) for _ in range(2)])
        tmp2 = Rot([A.alloc(512) for _ in range(2)])
        rstd = A.alloc(512)
        wr = A.alloc(16, 36)
        br = A.alloc(36)
        lgt = A.alloc(36)
        m8 = A.alloc(8)
        oh = A.alloc(4)
        lgm = A.alloc(4, 8)
        eg = A.alloc(4)
        sm = A.alloc(1)
        wg = A.alloc(1)
        oh1 = A.alloc(32)
        oh2 = A.alloc(32)
        p1 = A.alloc(1)
        p2 = A.alloc(1)
        dlt = A.alloc(1)
        S.dma(wr, I["wr"][l])
        S.dma(br, I["br"][l])
        py = Rot(PS[1:5])
        for (t0, n, v) in blocks:
            S.dma(mixb[:, :, :n], MIX[0:nmix, :, t0:t0 + n].rearrange("c p t -> p c t"))
            S.dma(xblk[:, :, :n], src[:, :, t0:t0 + n].rearrange("c p t -> p c t"))
            for dc in range(16):
                w = wb()
                S.dma(w, w_out[:, dc * 128:(dc + 1) * 128].rearrange("(kc p) n -> p kc n", p=128))
                ps = py()
                for kc in range(nmix):
                    S.mm(ps[:, :n], w[:, kc, :], mixb[:, kc, :n], start=(kc == 0), stop=(kc == nmix - 1))
                S.stt(xblk[:, dc, :n], ps[:, :n], mod_ap(l, 2, dc, v), xblk[:, dc, :n], ALU.mult, ALU.add)
            S.dma(XT[:, :, t0:t0 + n].rearrange("c p t -> p c t"), xblk[:, :, :n], q="pool")
            rms_mod(xblk, n, lambda j: AN[l][:, 1, j, v:v + 1], lambda j: mod_ap(l, 3, j, v), h2, tmp2, rstd, PS[0])
            S.dma(H2T[:, :, t0:t0 + n].rearrange("c p t -> p c t"), h2[:, :, :n], q="pool")
            for tt_ in range(n // 128):
                ti = t0 // 128 + tt_
                pr = PS[5 + tt_ % 2]
                for kc in range(16):
                    S.mm(pr[:, 0:36], h2[:, kc, tt_ * 128:(tt_ + 1) * 128], wr[:, kc, :],
                         start=(kc == 0), stop=(kc == 15))
                S.tt(lgt, pr[:, 0:36], br, ALU.add)
                S.reduce(sm, lgt[:, 0:4], ALU.max)
                S.ts(oh, lgt[:, 0:4], sm, None, ALU.is_equal)
                S.ts(eg, lgt[:, 0:4], sm, None, ALU.subtract)
                S.act(eg, eg, AF.Exp)
                S.reduce(wg, eg, ALU.add)
                S.recip(wg, wg)
                S.ts(oh, oh, -1.0, 1.0e4, ALU.add, ALU.mult)
                for g in range(4):
                    S.ts(lgm[:, g, :], lgt[:, 4 + 8 * g:12 + 8 * g], oh[:, g:g + 1], None, ALU.add)
                lgf = lgm.rearrange("p a b -> p (a b)")
                S.reduce(m8[:, 0:1], lgf, ALU.max)
                S.ts(oh1, lgf, m8[:, 0:1], None, ALU.is_equal)
                S.stt(oh2, oh1, -1.0e4, lgf, ALU.mult, ALU.add)
                S.reduce(m8[:, 1:2], oh2, ALU.max)
                S.ts(oh2, oh2, m8[:, 1:2], None, ALU.is_equal)
                S.tt(dlt, m8[:, 1:2], m8[:, 0:1], ALU.subtract)
                S.act(dlt, dlt, AF.Exp)
                S.ts(p1, dlt, 1.0, None, ALU.add)
                S.recip(p1, p1)
                S.tt(p2, dlt, p1, ALU.mult)
                S.tt(p1, p1, wg, ALU.mult)
                S.tt(p2, p2, wg, ALU.mult)
                S.ts(oh1, oh1, p1, None, ALU.mult)
                S.stt(GATES[:, ti, :], oh2, p2, oh1, ALU.mult, ALU.add)
        A.release(mk)

    def moe_stage(l, blocks):
        mk = A.mark()
        h2 = A.alloc(16, 512)
        acc = A.alloc(4, 2048)
        actT = A.alloc(8, 512)
        w13 = Rot([A.alloc(16, 128) for _ in range(4)])
        w2b = Rot([A.alloc(8, 512) for _ in range(2)])
        tmp = Rot([A.alloc(512) for _ in range(2)])
        pab = Rot([(PS[0], PS[1]), (PS[2], PS[3])])
        py = Rot(PS[4:8])
        W1 = I["moe_w1"]
        W3 = I["moe_w3"]
        W2 = I["moe_w2"]
        for (t0, n, v) in blocks:
            nt_ = n // 128
            S.dma(h2[:, :, :n], H2T[:, :, t0:t0 + n].rearrange("c p t -> p c t"))
            S.memset(acc, 0.0, eng="pool")
            for e in range(32):
                for ffc in range(8):
                    w1p = w13()
                    w3p = w13()
                    S.dma(w1p, W1[l, e, :, ffc * 128:(ffc + 1) * 128].rearrange("(kc p) n -> p kc n", p=128))
                    S.dma(w3p, W3[l, e, :, ffc * 128:(ffc + 1) * 128].rearrange("(kc p) n -> p kc n", p=128))
                    pa, pb = pab()
                    for kc in range(16):
                        S.mm(pa[:, :n], w1p[:, kc, :], h2[:, kc, :n], start=(kc == 0), stop=(kc == 15))
                    for kc in range(16):
                        S.mm(pb[:, :n], w3p[:, kc, :], h2[:, kc, :n], start=(kc == 0), stop=(kc == 15))
                    t = tmp()
                    S.act(t[:, :n], pa[:, :n], AF.Silu)
                    S.tt(actT[:, ffc, :n], t[:, :n], pb[:, :n], ALU.mult)
                for cb in range(4):
                    w2p = w2b()
                    S.dma(w2p, W2[l, e, :, cb * 512:(cb + 1) * 512].rearrange("(fc p) n -> p fc n", p=128))
                    for tt_ in range(nt_):
                        ps = py()
                        for fc in range(8):
                            S.mm(ps, actT[:, fc, tt_ * 128:(tt_ + 1) * 128], w2p[:, fc, :],
                                 start=(fc == 0), stop=(fc == 7))
                        av = acc[:, tt_, cb * 512:(cb + 1) * 512]
                        S.stt(av, ps, GATES[:, t0 // 128 + tt_, e:e + 1], av, ALU.mult, ALU.add)
            xblk = h2
            S.dma(xblk[:, :, :n], XT[:, :, t0:t0 + n].rearrange("c p t -> p c t"))
            for dc in range(16):
                pt = py()
                for tt_ in range(nt_):
                    S.transpose(pt[:, tt_ * 128:(tt_ + 1) * 128], acc[:, tt_, dc * 128:(dc + 1) * 128], ident)
                S.stt(xblk[:, dc, :n], pt[:, :n], mod_ap(l, 5, dc, v), xblk[:, dc, :n], ALU.mult, ALU.add)
            S.dma(XT[:, :, t0:t0 + n].rearrange("c p t -> p c t"), xblk[:, :, :n], q="pool")
        A.release(mk)

    def diff_stage():
        import math
        lam_init = 0.8 - 0.6 * math.exp(-0.3 * 1)
        mk = A.mark()
        qT = A.alloc(NLAT)
        kT = A.alloc(NT)
        V = A.alloc(18, 128)
        OUT = A.alloc(NLAT)
        dl = A.alloc(4, 64)
        pr = A.alloc(2, 64)
        lam = A.alloc(2)
        nlam = A.alloc(1)
        Pb = Rot([A.alloc(512) for _ in range(3)])
        r0 = A.alloc(512)
        o0 = A.alloc(512)
        o1 = A.alloc(512)
        sq = A.alloc(512)
        rstd = A.alloc(512)
        S.dma(dl, I["dlam"])
        S.tt(pr[:, 0, :], dl[:, 0, :], dl[:, 1, :], ALU.mult)
        S.tt(pr[:, 1, :], dl[:, 2, :], dl[:, 3, :], ALU.mult)
        S.reduce(lam[:, 0:1], pr[:, 0, :], ALU.add)
        S.reduce(lam[:, 1:2], pr[:, 1, :], ALU.add)
        S.act(lam, lam, AF.Exp)
        S.tt(nlam, lam[:, 1:2], lam[:, 0:1], ALU.subtract)
        S.ts(nlam, nlam, -lam_init, None, ALU.add)
        pss = Rot(PS[0:4])
        for h in range(8):
            S.dma(qT, FM[h, :, 0:NLAT])
            S.dma(kT, FM[8 + h])
            S.dma(V, TM[:, h * 128:(h + 1) * 128].rearrange("(j p) d -> p j d", p=128))
            for qb in range(4):
                q0 = qb * 512
                for c in range(2):
                    O = PS[4 + c]
                    Z = PS[6 + c]
                    for j in range(18):
                        ps = pss()
                        S.mm(ps, kT[c * 64:(c + 1) * 64, j * 128:(j + 1) * 128], qT[c * 64:(c + 1) * 64, q0:q0 + 512])
                        P = Pb()
                        S.act(P, ps, AF.Exp)
                        S.mm(O, V[:, j, :], P, start=(j == 0), stop=(j == 17))
                        S.mm(Z, ones, P, start=(j == 0), stop=(j == 17))
                S.recip(r0, PS[6])
                S.tt(o0, PS[4], r0, ALU.mult)
                S.recip(r0, PS[7])
                S.tt(o1, PS[5], r0, ALU.mult)
                S.stt(o0, o1, nlam, o0, ALU.mult, ALU.add)
                S.act(sq, o0, AF.Square)
                pm = pss()
                S.mm(pm, ones, sq)
                S.act(rstd, pm, AF.Ln, scale=1.0 / 128, bias=epsc)
                S.act(rstd, rstd, AF.Exp, scale=-0.5)
                S.stt(OUT[:, q0:q0 + 512], o0, 1.0 - lam_init, rstd, ALU.mult, ALU.mult)
            S.dma(MIX[h, :, 0:NLAT], OUT, q="pool")
        A.release(mk)

    def hgrn_stage():
        mk = A.mark()
        qT = A.alloc(NLAT)
        Kd = [A.alloc(NT), A.alloc(NT)]
        Gd = [A.alloc(NT), A.alloc(NT)]
        NGd = [A.alloc(NT), A.alloc(NT)]
        V = A.alloc(18, 128)
        onesr = A.alloc(NT)
        OUT = A.alloc(NLAT)
        LF = A.alloc(NT)
        Kt = [A.alloc(NT), A.alloc(NT)]
        Dt = [A.alloc(NT), A.alloc(NT)]
        eq = Rot([A.alloc(32) for _ in range(2)])
        Qt = [Rot([A.alloc(32) for _ in range(2)]), Rot([A.alloc(32) for _ in range(2)])]
        Pb = Rot([A.alloc(21, 32) for _ in range(2)])
        trilT = A.alloc(128)
        triuT = A.alloc(128)
        HG = A.alloc(512)
        sq = A.alloc(512)
        rstd = A.alloc(512)
        ob = Rot([A.alloc(512) for _ in range(2)])
        S.dma(trilT, I["trilT"])
        S.dma(triuT, I["triuT"])
        S.memset(onesr, 1.0)
        pscore = Rot([(PS[0], PS[1]), (PS[2], PS[3])])
        pout = Rot(PS[4:6])
        for h in range(8):
            S.dma(qT, FM[16 + h, :, 0:NLAT])
            S.dma(V, TM[:, 1024 + h * 128:1024 + (h + 1) * 128].rearrange("(j p) d -> p j d", p=128))
            S.dma(Kd[0], FM[24 + h])
            S.dma(LF, FM[32 + h])
            S.scan(Gd[0][:, NLAT:NT], onesr[:, 0:NCTX], LF[:, NLAT:NT], 0.0, ALU.mult, ALU.add)
            S.scan(Gd[0][:, 0:NLAT], onesr[:, 0:NLAT], LF[:, 0:NLAT], Gd[0][:, NT - 1:NT], ALU.mult, ALU.add)
            S.ts(NGd[0], Gd[0], -1.0, None, ALU.mult)
            S.dma(Kd[1], FM[40 + h])
            S.dma(LF, FM[48 + h])
            S.scan(Gd[1], onesr, LF, 0.0, ALU.mult, ALU.add)
            S.tt(Gd[1], LF, Gd[1], ALU.subtract)
            S.ts(NGd[1], Gd[1], -1.0, None, ALU.mult)
            for sb in range(64):
                t0 = 32 * sb
                Ii = t0 // 128
                sbi = sb % 4
                kblks = []
                qts = []
                for d in range(2):
                    if d == 0:
                        ai = t0 - 1 if t0 > 0 else NT - 1
                        slices = [(0, (Ii + 1) * 128), (NLAT, NT)]
                        tl = [16, 17] + list(range(0, Ii + 1))
                    else:
                        ai = t0 + 32
                        slices = [(Ii * 128, NT)]
                        tl = list(range(Ii, 16)) + [16, 17]
                    a = Gd[d][:, ai:ai + 1]
                    na = NGd[d][:, ai:ai + 1]
                    e_ = eq()
                    S.act(e_, Gd[d][:, t0:t0 + 32], AF.Exp, bias=na, scale=1.0)
                    q_ = Qt[d]()
                    S.tt(q_, qT[:, t0:t0 + 32], e_, ALU.mult)
                    qts.append(q_)
                    for (lo, hi) in slices:
                        S.ts(Dt[d][:, lo:hi], Gd[d][:, lo:hi], a, -60.0, ALU.subtract, ALU.max)
                        S.act(Dt[d][:, lo:hi], Dt[d][:, lo:hi], AF.Exp, scale=-1.0)
                        S.tt(Kt[d][:, lo:hi], Kd[d][:, lo:hi], Dt[d][:, lo:hi], ALU.mult, eng="pool")
                    kblks += [(d, j) for j in tl]
                assert len(kblks) == 21
                banks = pscore()
                for k, (d, j) in enumerate(kblks):
                    S.mm(banks[k // 16][:, (k % 16) * 32:(k % 16 + 1) * 32], Kt[d][:, j * 128:(j + 1) * 128], qts[d])
                P = Pb()
                Pf = P.rearrange("p a b -> p (a b)")
                S.copy(Pf[:, 0:512], banks[0][:, 0:512], eng="act")
                S.copy(Pf[:, 512:672], banks[1][:, 0:160], eng="act")
                kf = 2 + Ii
                kb = (Ii + 3)
                S.tt(P[:, kf, :], P[:, kf, :], trilT[:, 32 * sbi:32 * sbi + 32], ALU.mult)
                S.tt(P[:, kb, :], P[:, kb, :], triuT[:, 32 * sbi:32 * sbi + 32], ALU.mult)
                po = pout()
                for k, (d, j) in enumerate(kblks):
                    S.mm(po[:, 0:32], V[:, j, :], P[:, k, :], start=(k == 0), stop=(k == 20))
                S.copy(OUT[:, t0:t0 + 32], po[:, 0:32], eng="act")
            for qb in range(4):
                q0 = qb * 512
                S.dma(HG, FM[56 + h, :, q0:q0 + 512])
                S.act(sq, OUT[:, q0:q0 + 512], AF.Square)
                pm = PS[6 + qb % 2]
                S.mm(pm, ones, sq)
                S.act(rstd, pm, AF.Ln, scale=1.0 / 128, bias=epsc)
                S.act(rstd, rstd, AF.Exp, scale=-0.5)
                o = ob()
                S.tt(o, OUT[:, q0:q0 + 512], rstd, ALU.mult)
                S.tt(o, o, HG, ALU.mult)
                S.dma(MIX[8 + h, :, q0:q0 + 512], o, q="pool")
        A.release(mk)

    def final_stage():
        mk = A.mark()
        xblk = A.alloc(16, 512)
        ob = A.alloc(16, 512)
        tmp2 = Rot([A.alloc(512) for _ in range(2)])
        rstd = A.alloc(512)
        for (t0, n, v) in TOKBLOCKS[:4]:
            S.dma(xblk, XT[:, :, t0:t0 + n].rearrange("c p t -> p c t"))
            rms_mod(xblk, n, lambda j: NFW[:, j:j + 1], None, ob, tmp2, rstd, PS[0])
            S.dma(outT[:, :, t0:t0 + n].rearrange("c p t -> p c t"), ob, q="pool", is_output=True)
        A.release(mk)

    ALL = ["adaln0", "inproj0", "na0", "ret0", "post0", "moe0", "adaln1", "inproj1", "diff1", "hgrn1", "post1", "moe1", "final"]
    st = stages or ALL
    FM0 = [(0, 8, 0, "scale", 128 ** -0.5), (1024, 8, 8, "scale", 1.0), (3072, 8, 16, "rope", 1.0),
           (4096, 8, 24, "rope", 128 ** -0.5), (7168, 16, 32, "silu", None)]
    TM0 = [(2048, 1024, 0), (5120, 2048, 1024)]
    FM1 = [(0, 8, 0, "rope", 64 ** -0.5), (1024, 8, 8, "rope", 1.0), (3072, 8, 16, "silu", None),
           (4096, 8, 24, "gate", 32), (5120, 8, 40, "gate", 48), (7168, 8, 56, "silu", None)]
    TM1 = [(2048, 1024, 0), (6144, 1024, 1024)]
    if "adaln0" in st:
        adaln(0)
    if "inproj0" in st:
        inproj(0, I["xin"], I["w_in_even"], FM0, TM0, ("cos128", "sin128", "perm128"))
    if "na0" in st:
        na_stage()
    if "ret0" in st:
        ret_stage()
    if "post0" in st:
        post_stage(0, I["xin"], I["w_out_even"], 24, TOKBLOCKS)
    if "dbggates" in st:
        S.dma(DBG[:, 256:256 + 576], GATES.rearrange("p a b -> p (a b)"), q="pool")
    if "moe0" in st:
        moe_stage(0, TOKBLOCKS)
    if "adaln1" in st:
        adaln(1)
    if "inproj1" in st:
        inproj(1, XT, I["w_in_odd"], FM1, TM1, ("cos64", "sin64", "perm64"))
    if "diff1" in st:
        diff_stage()
    if "hgrn1" in st:
        hgrn_stage()
    if "post1" in st:
        post_stage(1, XT, I["w_out_odd"], 16, TOKBLOCKS[:4])
    if "dbggates1" in st:
        S.dma(DBG[:, 1024:1024 + 576], GATES.rearrange("p a b -> p (a b)"), q="pool")
    if "moe1" in st:
        moe_stage(1, TOKBLOCKS[:4])
    if "final" in st:
        final_stage()
    if "dbgmod" in st:
        S.dma(DBG[:, 0:192], MOD[0].rearrange("p a b -> p (a b)"), q="pool")
        S.dma(DBG[:, 192:256], AN[0].rearrange("p a b c -> p (a b c)"), q="pool")
    S.emit()
    stack.close()
    return nc


def kernel(**inputs):
    maps = host_prep(inputs)
    nc = build_program()
    res = run_bass_kernel_spmd(nc, maps, core_ids=list(range(8)))
    outs = []
    for r in res.results:
        o = np.asarray(r["outT"], dtype=np.float32).reshape(D, NLAT)
        outs.append(o.T)
    return np.ascontiguousarray(np.stack(outs, axis=0), dtype=np.float32)
```

```python
import numpy as np
import concourse.bass as bass
import concourse.mybir as mybir
from concourse.bass_utils import run_bass_kernel_spmd
from contextlib import ExitStack

F32 = mybir.dt.float32
BF16 = mybir.dt.bfloat16
AF = mybir.ActivationFunctionType
ALU = mybir.AluOpType
AX = mybir.AxisListType
DT_SIZE = {F32: 4, BF16: 2, mybir.dt.int32: 4, mybir.dt.uint32: 4, mybir.dt.float32r: 4}

SEM_LIM = 30000
DMA_SLOTS = 6


class Sched:
    ENGS = ("pe", "act", "dve", "pool", "sp")

    def __init__(self, nc, stack):
        self.nc = nc
        self.stack = stack
        self.ops = {e: [] for e in self.ENGS}
        self.cnt = {e: 0 for e in self.ENGS}
        self.sems = {}
        self.acc = {}
        self.dma_slots = {}
        self.dma_i = {q: 0 for q in self.ENGS}
        self.nsem = 0
        self.out_sigs = []

    def _newsem(self):
        self.nsem += 1
        return self.stack.enter_context(self.nc.semaphore("s%d" % self.nsem))

    def _sig_compute(self, eng):
        k = self.cnt[eng]
        self.cnt[eng] += 1
        seg, val = k // SEM_LIM, k % SEM_LIM + 1
        if (eng, seg) not in self.sems:
            self.sems[(eng, seg)] = self._newsem()
        return (self.sems[(eng, seg)], val)

    @staticmethod
    def _interval(ap):
        t = ap.tensor
        name = t.name
        esz = DT_SIZE[ap.dtype]
        dims = ap.ap
        off = ap.offset
        space = str(ap.space)
        if space in ("SB", "PSUM"):
            pstep = dims[0][0]
            lo = off % pstep if pstep > 0 else off
            rest = dims[1:]
        else:
            lo = off
            rest = dims
        span = 0
        for st, n in rest:
            span += abs(st) * (n - 1)
        return name, lo * esz, (lo + span + 1) * esz

    def _deps(self, reads, writes, sig, eng):
        deps = []
        for ap, is_w in [(a, False) for a in reads] + [(a, True) for a in writes]:
            name, lo, hi = self._interval(ap)
            lst = self.acc.setdefault(name, [])
            keep = []
            for ent in lst:
                elo, ehi, esig, ew, eeng = ent
                if esig is sig:
                    keep.append(ent)
                    continue
                ov = elo < hi and lo < ehi
                if ov and (ew or is_w):
                    deps.append((esig, eeng))
                contained = lo <= elo and ehi <= hi
                if contained and (is_w or (not ew and eeng == eng and eng != "dma")):
                    continue
                keep.append(ent)
            keep.append([lo, hi, sig, is_w, eng])
            self.acc[name] = keep
        return deps

    def op(self, eng, fn, reads, writes):
        sig = self._sig_compute(eng)
        deps = self._deps(reads, writes, sig, eng)
        self.ops[eng].append((fn, sig, deps, False))
        return sig

    def dma(self, out, in_, q="sp", is_output=False, **kw):
        slots = self.dma_slots.setdefault(q, [[None, 0] for _ in range(DMA_SLOTS)])
        i = self.dma_i[q]
        self.dma_i[q] += 1
        slot = slots[i % DMA_SLOTS]
        deps = []
        if slot[0] is None or slot[1] >= SEM_LIM // 16:
            if slot[0] is not None:
                deps.append(((slot[0], slot[1] * 16), "dma"))
            slot[0] = self._newsem()
            slot[1] = 0
        if slot[1] > 0:
            deps.append(((slot[0], slot[1] * 16), "dma"))
        slot[1] += 1
        sig = (slot[0], slot[1] * 16)
        deps += self._deps([in_], [out], sig, "dma")
        fn = lambda e, out=out, in_=in_, kw=kw: e.dma_start(out=out, in_=in_, **kw)
        self.ops[q].append((fn, sig, deps, True))
        if is_output:
            self.out_sigs.append(sig)
        return sig

    def dma_fn(self, fn, reads, writes, q="sp", max_inflight=None):
        slots = self.dma_slots.setdefault(q, [[None, 0] for _ in range(DMA_SLOTS)])
        i = self.dma_i[q]
        self.dma_i[q] += 1
        slot = slots[i % DMA_SLOTS]
        deps = []
        if slot[0] is None or slot[1] >= SEM_LIM // 16:
            if slot[0] is not None:
                deps.append(((slot[0], slot[1] * 16), "dma"))
            slot[0] = self._newsem()
            slot[1] = 0
        if slot[1] > 0:
            deps.append(((slot[0], slot[1] * 16), "dma"))
        slot[1] += 1
        sig = (slot[0], slot[1] * 16)
        deps += self._deps(reads, writes, sig, "dma")
        rec = self.__dict__.setdefault("recent", {}).setdefault(q, [])
        if max_inflight is not None and len(rec) >= max_inflight:
            deps.append((rec[-max_inflight], "dma"))
        rec.append(sig)
        del rec[:-8]
        self.ops[q].append((fn, sig, deps, True))
        return sig

    def emit(self):
        nc = self.nc
        final_waits = []
        for q, slots in self.dma_slots.items():
            for s, c in slots:
                if s is not None and c > 0:
                    final_waits.append((s, c * 16))
        with nc.Block() as block:
            def run(eng_name, e):
                known = {}
                for fn, sig, deps, is_dma in self.ops[eng_name]:
                    for (dsem, dval), deng in deps:
                        if deng == "pe" and eng_name == "pe" and not is_dma:
                            continue
                        key = id(dsem)
                        if known.get(key, 0) >= dval:
                            continue
                        e.wait_ge(dsem, dval)
                        known[key] = dval
                    ins = fn(e)
                    ins.then_inc(sig[0], 16 if is_dma else 1)
                if eng_name == "sp":
                    for s, v in final_waits:
                        e.wait_ge(s, v)

            @block.tensor
            def _(e):
                run("pe", e)

            @block.scalar
            def _(e):
                run("act", e)

            @block.vector
            def _(e):
                run("dve", e)

            @block.gpsimd
            def _(e):
                run("pool", e)

            @block.sync
            def _(e):
                run("sp", e)

    def mm(self, out, lhsT, rhs, start=True, stop=True):
        return self.op("pe", lambda e: e.matmul(out, lhsT, rhs, start=start, stop=stop),
                       [lhsT, rhs], [out])

    def transpose(self, out, in_, ident):
        return self.op("pe", lambda e: e.transpose(out, in_, ident), [in_, ident], [out])

    def act(self, out, in_, func, bias=None, scale=None, accum_out=None, eng="act"):
        reads = [in_]
        kw = {}
        if bias is not None:
            kw["bias"] = bias
            if not isinstance(bias, (int, float)):
                reads.append(bias)
        if scale is not None:
            kw["scale"] = scale
            if not isinstance(scale, (int, float)):
                reads.append(scale)
        writes = [out]
        if accum_out is not None:
            kw["accum_out"] = accum_out
            writes.append(accum_out)
        return self.op(eng, lambda e: e.activation(out=out, in_=in_, func=func, **kw), reads, writes)

    def tt(self, out, in0, in1, op, eng="dve"):
        return self.op(eng, lambda e: e.tensor_tensor(out, in0, in1, op), [in0, in1], [out])

    def ts(self, out, in0, s1, s2=None, op0=ALU.mult, op1=None, eng="dve", accum_out=None):
        reads = [in0]
        for s in (s1, s2):
            if s is not None and not isinstance(s, (int, float)):
                reads.append(s)
        writes = [out]
        kw = {}
        if op1 is not None:
            kw["op1"] = op1
        if accum_out is not None:
            kw["accum_out"] = accum_out
            writes.append(accum_out)
        return self.op(eng, lambda e: e.tensor_scalar(out, in0, s1, s2, op0, **kw), reads, writes)

    def stt(self, out, in0, scalar, in1, op0, op1, eng="dve"):
        reads = [in0, in1]
        if not isinstance(scalar, (int, float)):
            reads.append(scalar)
        return self.op(eng, lambda e: e.scalar_tensor_tensor(out, in0, scalar, in1, op0, op1), reads, [out])

    def copy(self, out, in_, eng="dve"):
        if eng == "act":
            return self.act(out, in_, AF.Copy)
        return self.op(eng, lambda e: e.tensor_copy(out, in_), [in_], [out])

    def memset(self, ap, val, eng="dve"):
        return self.op(eng, lambda e: e.memset(ap, val), [], [ap])

    def reduce(self, out, in_, op=ALU.add, axis=AX.X, eng="dve"):
        return self.op(eng, lambda e: e.tensor_reduce(out, in_, axis, op), [in_], [out])

    def recip(self, out, in_):
        return self.op("dve", lambda e: e.reciprocal(out, in_), [in_], [out])

    def scan(self, out, d0, d1, initial, op0, op1):
        reads = [d0, d1]
        if not isinstance(initial, (int, float)):
            reads.append(initial)
        return self.op("dve", lambda e: e.tensor_tensor_scan(out, d0, d1, initial, op0, op1), reads, [out])

    def max8(self, out, in_):
        return self.op("dve", lambda e: e.max(out, in_), [in_], [out])

D = 2048
NT = 2304
NLAT = 2048
NCTX = 256
EPS = 1e-6
NEG = -30000.0
ARENA = 47 * 1024

LL_OFF = 1920
LL_U = 3968
LC_OFF = 128
LC_U = 2176


def _rope_tables(dim, nheadrep):
    n_f = dim // 4
    inv = (10000.0 ** (-np.arange(n_f, dtype=np.float32) / n_f)).astype(np.float32)
    t = np.arange(NLAT)
    pos = np.stack([t // 64, t % 64], axis=-1).astype(np.float32)
    ang = (pos[:, :, None] * inv).astype(np.float32)
    cos = np.cos(ang).astype(np.float32)
    sin = np.sin(ang).astype(np.float32)
    C = np.zeros((128, NLAT), np.float32)
    S = np.zeros((128, NLAT), np.float32)
    PM = np.zeros((128, 128), np.float32)
    for p in range(128):
        dd = p % dim
        axis = dd // (dim // 2)
        half = (dd % (dim // 2)) // n_f
        f = dd % n_f
        C[p] = cos[:, axis, f]
        S[p] = sin[:, axis, f] * (-1.0 if half == 0 else 1.0)
        partner = p + n_f if half == 0 else p - n_f
        PM[partner, p] = 1.0
    return C, S, PM


def _na_bias(rpb):
    out = np.full((8, 7, 128, 7, 128), NEG, np.float32)
    type_i = [0, 1, 2, 3, 13, 14, 15]
    b = np.arange(128)[:, None]
    n = np.arange(128)[None, :]
    for ti, i in enumerate(type_i):
        t = 128 * i + n
        r = t // 64
        c = t % 64
        rs = np.clip(r - 4, 0, 24)
        cs = np.clip(c - 8, 0, 48)
        for jj in range(7):
            j = i - 3 + jj
            if j < 0 or j > 15:
                continue
            s = 128 * j + b
            kr = s // 64
            kc = s % 64
            ok = (kr >= rs) & (kr < rs + 8) & (kc >= cs) & (kc < cs + 16)
            ro = np.clip(kr - r + 7, 0, 14)
            co = np.clip(kc - c + 15, 0, 30)
            ro, co, ok = np.broadcast_arrays(ro, co, ok)
            for h in range(8):
                vals = rpb[h][ro, co]
                out[h, ti, :, jj, :] = np.where(ok, vals, np.float32(NEG))
    return out


def _na_type(i):
    if i < 3:
        return i
    if i <= 12:
        return 3
    return i - 9


def _const_tables():
    C = {}
    C["ones"] = np.ones((128, 128), np.float32)
    C["ident"] = np.eye(128, dtype=np.float32)
    c1, s1, p1 = _rope_tables(128, 1)
    c2, s2, p2 = _rope_tables(64, 2)
    C["cos128"], C["sin128"], C["perm128"] = c1, s1, p1
    C["cos64"], C["sin64"], C["perm64"] = c2, s2, p2
    b = np.arange(128, dtype=np.float64)[:, None]
    u = np.arange(LL_U, dtype=np.float64)[None, :]
    E = u - b - LL_OFF
    BIG = 1.0e7
    C["epm"] = np.where(E >= 0, E, BIG).astype(np.float32)
    C["enm"] = np.where(E <= 0, -E, BIG).astype(np.float32)
    u2 = np.arange(LC_U, dtype=np.float64)[None, :]
    dl = u2 - b - LC_OFF
    C["ec1"] = (dl + 256.0).astype(np.float32)
    C["ec2"] = (2048.0 - dl).astype(np.float32)
    n = np.arange(128)[None, :]
    bb = np.arange(128)[:, None]
    C["trilT"] = (bb <= n).astype(np.float32)
    C["triuT"] = (bb >= n).astype(np.float32)
    C["sltT"] = (bb < n).astype(np.float32)
    C["qp"] = (np.arange(4)[None, :] * 128 + np.arange(128)[:, None]).astype(np.float32)
    return C


def host_prep(inputs):
    f32 = np.float32
    g = {k: np.ascontiguousarray(np.asarray(v, dtype=f32)) for k, v in inputs.items()}
    shared = {}
    shared["ada_w"] = g["ada_w"]
    shared["ada_b"] = np.ascontiguousarray(g["ada_b"].reshape(2, 96, 128).transpose(0, 2, 1))
    shared["n1w"] = np.ascontiguousarray(g["norm1_w"].reshape(2, 16, 128).transpose(0, 2, 1))
    shared["n2w"] = np.ascontiguousarray(g["norm2_w"].reshape(2, 16, 128).transpose(0, 2, 1))
    shared["nfw"] = np.ascontiguousarray(g["norm_f_w"].reshape(16, 128).T)
    shared["w_in_even"] = g["w_in_even"][0]
    shared["w_out_even"] = g["w_out_even"][0]
    shared["w_in_odd"] = g["w_in_odd"][0]
    shared["w_out_odd"] = g["w_out_odd"][0]
    shared["nabias"] = _na_bias(g["na_rpb"][0])
    shared["retdec"] = np.ascontiguousarray(np.broadcast_to(g["ret_decay"][0].reshape(1, 16), (128, 16)))
    shared["dlam"] = np.ascontiguousarray(np.broadcast_to(g["diff_lambda"][0].reshape(1, 4, 64), (128, 4, 64)))
    shared["hglb"] = np.ascontiguousarray(g["hg_lb_logits"].reshape(2, 8, 128).transpose(2, 0, 1))
    wr = np.concatenate([g["router_g_w"], g["router_e_w"]], axis=-1)
    shared["wr"] = np.ascontiguousarray(wr.reshape(2, 16, 128, 36).transpose(0, 2, 1, 3))
    br = np.concatenate([g["router_g_b"], g["router_e_b"]], axis=-1)
    shared["br"] = np.ascontiguousarray(np.broadcast_to(br[:, None, :], (2, 128, 36)))
    def _w13(w):
        return np.ascontiguousarray(w.reshape(2, 32, 16, 128, 4, 256).transpose(0, 1, 4, 3, 2, 5)).reshape(2 * 32 * 4 * 128, 4096)
    shared["moe_w1"] = _w13(g["moe_w1"])
    shared["moe_w3"] = _w13(g["moe_w3"])
    shared["moe_w2"] = np.ascontiguousarray(
        g["moe_w2"].reshape(2, 32, 8, 128, 4, 512).transpose(0, 1, 4, 3, 2, 5)).reshape(2 * 32 * 4 * 128, 4096)
    shared.update(_const_tables())
    per_core = []
    for bi in range(g["x"].shape[0]):
        xin = np.concatenate([g["x"][bi].T, g["ctx"][bi].T], axis=1)
        cT = np.stack([g["c"][bi].reshape(16, 128).T, g["c_ctx"].reshape(16, 128).T], axis=-1)
        m = dict(shared)
        m["xin"] = np.ascontiguousarray(xin.reshape(16, 128, NT))
        m["cT"] = np.ascontiguousarray(cT)
        per_core.append(m)
    return per_core


INPUT_SHAPES = {
    "xin": [16, 128, NT], "cT": [128, 16, 2], "ada_w": [2, D, 6 * D], "ada_b": [2, 128, 96],
    "n1w": [2, 128, 16], "n2w": [2, 128, 16], "nfw": [128, 16],
    "w_in_even": [D, 9216], "w_out_even": [3072, D], "w_in_odd": [D, 8192], "w_out_odd": [D, D],
    "nabias": [8, 7, 128, 7, 128], "retdec": [128, 16], "dlam": [128, 4, 64], "hglb": [128, 2, 8],
    "wr": [2, 128, 16, 36], "br": [2, 128, 36],
    "moe_w1": [32768, 4096], "moe_w3": [32768, 4096], "moe_w2": [32768, 4096], "sltT": [128, 128], "qp": [128, 4],
    "ones": [128, 128], "ident": [128, 128], "cos128": [128, NLAT], "sin128": [128, NLAT], "perm128": [128, 128],
    "cos64": [128, NLAT], "sin64": [128, NLAT], "perm64": [128, 128],
    "epm": [128, LL_U], "enm": [128, LL_U], "ec1": [128, LC_U], "ec2": [128, LC_U],
    "trilT": [128, 128], "triuT": [128, 128],
}


class Arena:
    def __init__(self, ap, size):
        self.ap = ap
        self.size = size
        self.off = 0

    def alloc(self, *free):
        n = 1
        for f in free:
            n *= f
        assert self.off + n <= self.size, ("arena overflow", self.off, n, self.size)
        v = self.ap[:, self.off:self.off + n]
        self.off += n
        if len(free) == 2:
            v = v.rearrange("p (a b) -> p a b", a=free[0])
        elif len(free) == 3:
            v = v.rearrange("p (a b c) -> p a b c", a=free[0], b=free[1])
        return v

    def mark(self):
        return self.off

    def release(self, m):
        self.off = m


TOKBLOCKS = [(0, 512, 0), (512, 512, 0), (1024, 512, 0), (1536, 512, 0), (2048, 256, 1)]


class Rot:
    def __init__(self, items):
        self.items = list(items)
        self.i = 0

    def __call__(self):
        x = self.items[self.i % len(self.items)]
        self.i += 1
        return x


def build_program(dbg=None, stages=None, use_bf16_moe=False, tiny=()):
    dbg = dbg or ()
    nc = bass.Bass("TRN2", target_bir_lowering=False)
    stack = ExitStack()
    I = {k: nc.dram_tensor(k, ([1] * len(shp) if k in tiny else shp), F32, kind="ExternalInput").ap() for k, shp in INPUT_SHAPES.items()}
    outT = nc.dram_tensor("outT", [16, 128, NLAT], F32, kind="ExternalOutput").ap()

    def scratch(name, shape):
        kind = "ExternalOutput" if name in dbg else "Internal"
        return nc.dram_tensor(name, shape, F32, kind=kind).ap()

    XT = scratch("XT", [16, 128, NT])
    H2T = scratch("H2T", [16, 128, NT])
    FM = scratch("FM", [64, 128, NT])
    TM = scratch("TM", [NT, 3072])
    MIX = scratch("MIX", [24, 128, NT])
    DBG = scratch("DBG", [128, 4096])
    XS = scratch("XS", [68 * 128, D])
    YB = scratch("YB", [68 * 128, D])

    SBt = stack.enter_context(nc.sbuf_tensor("SB", [128, ARENA], F32))
    PS = [stack.enter_context(nc.psum_tensor("ps%d" % i, [128, 512], F32))[:, :] for i in range(8)]
    S = Sched(nc, stack)
    A = Arena(SBt, ARENA)

    ones = A.alloc(128)
    ident = A.alloc(128)
    sc = A.alloc(16, 2)
    MOD = [A.alloc(96, 2) for _ in range(2)]
    AN = [A.alloc(2, 16, 2) for _ in range(2)]
    NW = [[A.alloc(16) for _ in range(2)] for _ in range(2)]
    NFW = A.alloc(16)
    OH1A = A.alloc(18, 32)
    OH2A = A.alloc(18, 32)
    G12 = A.alloc(18, 2)
    epsc = A.alloc(1)
    S.memset(epsc, EPS)
    S.dma(ones, I["ones"])
    S.dma(ident, I["ident"])
    S.dma(sc, I["cT"])
    S.dma(NFW, I["nfw"])
    for l in range(2):
        S.dma(NW[l][0], I["n1w"][l])
        S.dma(NW[l][1], I["n2w"][l])
    S.act(sc, sc, AF.Silu)

    def mod_ap(l, m, j, v):
        return MOD[l][:, m * 16 + j, v:v + 1]

    def adaln(l):
        mk = A.mark()
        adab = A.alloc(96)
        S.dma(adab, I["ada_b"][l])
        wbuf = Rot([A.alloc(16, 128) for _ in range(3)])
        pr = Rot(PS[0:4])
        for j in range(96):
            w = wbuf()
            S.dma(w, I["ada_w"][l, :, j * 128:(j + 1) * 128].rearrange("(kc p) n -> p kc n", p=128))
            ps = pr()
            for kc in range(16):
                S.mm(ps[:, 0:2], w[:, kc, :], sc[:, kc, :], start=(kc == 0), stop=(kc == 15))
            S.act(MOD[l][:, j, :], ps[:, 0:2], AF.Identity, bias=adab[:, j:j + 1])
        for ni, m in ((0, 1), (1, 4)):
            for v in range(2):
                S.stt(AN[l][:, ni, :, v], MOD[l][:, m * 16:(m + 1) * 16, v], 1.0, NW[l][ni], ALU.add, ALU.mult)
        A.release(mk)

    def rms_mod(xblk, n, scale_ap, bias_ap, out, tmp2, rstd, psb):
        for j in range(16):
            t = tmp2()
            S.act(t[:, :n], xblk[:, j, :n], AF.Square)
            S.mm(psb[:, :n], ones, t[:, :n], start=(j == 0), stop=(j == 15))
        S.act(rstd[:, :n], psb[:, :n], AF.Ln, scale=1.0 / 2048, bias=epsc)
        S.act(rstd[:, :n], rstd[:, :n], AF.Exp, scale=-0.5)
        for j in range(16):
            t = tmp2()
            S.tt(t[:, :n], xblk[:, j, :n], rstd[:, :n], ALU.mult)
            b = bias_ap(j) if bias_ap is not None else 0.0
            S.act(out[:, j, :n], t[:, :n], AF.Identity, scale=scale_ap(j), bias=b)

    def inproj(l, src, w_in, fm_specs, tm_specs, rope):
        mk = A.mark()
        xblk = A.alloc(16, 512)
        hT = A.alloc(16, 512)
        tmp2 = Rot([A.alloc(512) for _ in range(2)])
        rstd = A.alloc(512)
        wfm = Rot([A.alloc(16, 128) for _ in range(3)])
        wtm = Rot([A.alloc(16, 256) for _ in range(2)])
        obuf = Rot([A.alloc(512) for _ in range(4)])
        tbuf = Rot([A.alloc(512) for _ in range(4)])
        otm = Rot([A.alloc(256) for _ in range(3)])
        if rope is not None:
            cosT = A.alloc(NLAT)
            sinT = A.alloc(NLAT)
            perm = A.alloc(128)
            S.dma(cosT, I[rope[0]])
            S.dma(sinT, I[rope[1]])
            S.dma(perm, I[rope[2]])
        if l == 1:
            lbt = A.alloc(2, 8)
            lb = A.alloc(8)
            oml = A.alloc(8)
            S.dma(lbt, I["hglb"])
            S.tt(lb, lbt[:, 1, :], lbt[:, 0, :], ALU.subtract)
            S.act(lb, lb, AF.Sigmoid)
            S.ts(oml, lb, -1.0, 1.0, ALU.mult, ALU.add)
        pfm = Rot(PS[1:5])
        pperm = Rot(PS[5:7])
        ptm = Rot(PS[5:8])
        for (t0, n, v) in TOKBLOCKS:
            S.dma(xblk[:, :, :n], src[:, :, t0:t0 + n].rearrange("c p t -> p c t"))
            rms_mod(xblk, n, lambda j: AN[l][:, 0, j, v:v + 1], lambda j: mod_ap(l, 0, j, v), hT, tmp2, rstd, PS[0])
            for (col0, nch, dst0, kind, arg) in fm_specs:
                for ci in range(nch):
                    w = wfm()
                    c0 = col0 + ci * 128
                    S.dma(w, w_in[:, c0:c0 + 128].rearrange("(kc p) n -> p kc n", p=128))
                    ps = pfm()
                    for kc in range(16):
                        S.mm(ps[:, :n], w[:, kc, :], hT[:, kc, :n], start=(kc == 0), stop=(kc == 15))
                    o = obuf()
                    if kind == "scale":
                        S.act(o[:, :n], ps[:, :n], AF.Copy, scale=arg)
                    elif kind == "silu":
                        S.act(o[:, :n], ps[:, :n], AF.Silu)
                    elif kind == "rope":
                        if v == 1:
                            S.act(o[:, :n], ps[:, :n], AF.Copy, scale=arg)
                        else:
                            xs = tbuf()
                            S.act(xs[:, :n], ps[:, :n], AF.Copy, scale=arg)
                            p2 = pperm()
                            S.mm(p2[:, :n], perm, xs[:, :n])
                            t2 = tbuf()
                            S.tt(t2[:, :n], p2[:, :n], sinT[:, t0:t0 + n], ALU.mult)
                            S.tt(xs[:, :n], xs[:, :n], cosT[:, t0:t0 + n], ALU.mult, eng="pool")
                            S.tt(o[:, :n], xs[:, :n], t2[:, :n], ALU.add)
                    elif kind == "gate":
                        f = tbuf()
                        S.act(f[:, :n], ps[:, :n], AF.Sigmoid)
                        S.ts(f[:, :n], f[:, :n], oml[:, ci:ci + 1], lb[:, ci:ci + 1], ALU.mult, ALU.add)
                        S.ts(o[:, :n], f[:, :n], -1.0, 1.0, ALU.mult, ALU.add)
                        o2 = obuf()
                        S.act(o2[:, :n], f[:, :n], AF.Ln)
                        S.dma(FM[arg + ci, :, t0:t0 + n], o2[:, :n], q="pool")
                    S.dma(FM[dst0 + ci, :, t0:t0 + n], o[:, :n], q="pool")
            for (col0, ncols, tc0) in tm_specs:
                for pc in range(ncols // 256):
                    w = wtm()
                    c0 = col0 + pc * 256
                    S.dma(w, w_in[:, c0:c0 + 256].rearrange("(kc p) n -> p kc n", p=128))
                    for tt_ in range(n // 128):
                        ps = ptm()
                        for kc in range(16):
                            S.mm(ps[:, :256], hT[:, kc, tt_ * 128:(tt_ + 1) * 128], w[:, kc, :],
                                 start=(kc == 0), stop=(kc == 15))
                        o = otm()
                        S.copy(o, ps[:, :256], eng="act" if tt_ % 2 else "dve")
                        r0 = t0 + tt_ * 128
                        S.dma(TM[r0:r0 + 128, tc0 + pc * 256: tc0 + (pc + 1) * 256], o, q="pool")
        A.release(mk)

    def na_stage():
        mk = A.mark()
        qT = A.alloc(NT)
        kT = A.alloc(NT)
        V = A.alloc(18, 128)
        OUT = A.alloc(NT)
        bias = Rot([A.alloc(7, 128) for _ in range(2)])
        Pb = Rot([A.alloc(9, 128) for _ in range(2)])
        rz = Rot([A.alloc(256) for _ in range(2)])
        psS = Rot([(PS[0], PS[1], PS[2]), (PS[3], PS[4], PS[5])])
        for h in range(8):
            S.dma(qT, FM[h])
            S.dma(kT, FM[8 + h])
            S.dma(V, TM[:, h * 128:(h + 1) * 128].rearrange("(j p) d -> p j d", p=128))
            for i in range(16):
                jl = [j for j in range(i - 3, i + 4) if 0 <= j <= 15]
                jj0 = jl[0] - (i - 3)
                bt = bias()
                S.dma(bt, I["nabias"][h, _na_type(i)])
                keys = jl + [16, 17]
                nk = len(keys)
                banks = psS()
                P = Pb()

                def sc_ap(idx):
                    return banks[idx // 4][:, (idx % 4) * 128:(idx % 4 + 1) * 128]
                for idx, j in enumerate(keys):
                    S.mm(sc_ap(idx), kT[:, j * 128:(j + 1) * 128], qT[:, i * 128:(i + 1) * 128])
                nl = len(jl)
                idx = 0
                while idx < nl:
                    g = min(4 - idx % 4, nl - idx)
                    bk = banks[idx // 4]
                    c0 = (idx % 4) * 128
                    S.tt(P[:, idx:idx + g, :], bk[:, c0:c0 + g * 128].rearrange("p (a b) -> p a b", a=g),
                         bt[:, jj0 + idx:jj0 + idx + g, :], ALU.add)
                    idx += g
                S.act(P[:, 0:nl, :], P[:, 0:nl, :], AF.Exp)
                for idx in range(nl, nk):
                    S.act(P[:, idx, :], sc_ap(idx), AF.Exp)
                po = PS[6]
                pz = PS[7]
                for idx, j in enumerate(keys):
                    S.mm(po[:, 0:128], V[:, j, :], P[:, idx, :], start=(idx == 0), stop=(idx == nk - 1))
                for idx, j in enumerate(keys):
                    S.mm(pz[:, 0:128], ones, P[:, idx, :], start=(idx == 0), stop=(idx == nk - 1))
                r = rz()
                S.recip(r[:, 0:128], pz[:, 0:128])
                S.tt(OUT[:, i * 128:(i + 1) * 128], po[:, 0:128], r[:, 0:128], ALU.mult)
            banks = psS()
            P = Pb()
            Pf = P.rearrange("p a b -> p (a b)")
            for idx, j in enumerate((16, 17)):
                S.mm(banks[idx][:, 0:256], kT[:, j * 128:(j + 1) * 128], qT[:, NLAT:NT])
                S.act(Pf[:, 256 * idx:256 * idx + 256], banks[idx][:, 0:256], AF.Exp)
            po = PS[6]
            pz = PS[7]
            for idx, j in enumerate((16, 17)):
                pv = Pf[:, 256 * idx:256 * idx + 256]
                S.mm(po[:, 0:256], V[:, j, :], pv, start=(idx == 0), stop=(idx == 1))
            for idx, j in enumerate((16, 17)):
                pv = Pf[:, 256 * idx:256 * idx + 256]
                S.mm(pz[:, 0:256], ones, pv, start=(idx == 0), stop=(idx == 1))
            r = rz()
            S.recip(r, pz[:, 0:256])
            S.tt(OUT[:, NLAT:NT], po[:, 0:256], r, ALU.mult)
            S.dma(MIX[h], OUT, q="pool")
        A.release(mk)

    def ret_stage():
        mk = A.mark()
        qT = A.alloc(NT)
        kT = A.alloc(NT)
        V = A.alloc(18, 256)
        MLL = A.alloc(LL_U)
        MLC = A.alloc(LC_U)
        tabA = A.alloc(LL_U)
        tabB = A.alloc(LL_U)
        lg = A.alloc(16)
        RG = A.alloc(2, 512)
        Pb = Rot([A.alloc(512) for _ in range(3)])
        sq = Rot([A.alloc(512) for _ in range(2)])
        rstd = A.alloc(512)
        ob = Rot([A.alloc(512) for _ in range(3)])
        S.dma(lg, I["retdec"])
        S.act(lg, lg, AF.Exp)
        S.ts(lg, lg, -1.0, None, ALU.mult)
        pss = Rot(PS[0:4])
        for h in range(8):
            S.dma(qT, FM[16 + h])
            S.dma(kT, FM[24 + h])
            S.dma(V, TM[:, 1024 + h * 256:1024 + (h + 1) * 256].rearrange("(j p) d -> p j d", p=128))
            lgf = lg[:, h:h + 1]
            lgb = lg[:, 8 + h:9 + h]
            S.dma(tabA, I["epm"])
            S.dma(tabB, I["enm"])
            S.act(MLL, tabA, AF.Exp, scale=lgf)
            S.act(tabB, tabB, AF.Exp, scale=lgb)
            S.tt(MLL, MLL, tabB, ALU.add)
            S.dma(tabA[:, 0:LC_U], I["ec1"])
            S.dma(tabB[:, 0:LC_U], I["ec2"])
            S.act(MLC, tabA[:, 0:LC_U], AF.Exp, scale=lgf)
            S.act(tabB[:, 0:LC_U], tabB[:, 0:LC_U], AF.Exp, scale=lgb)
            S.tt(MLC, MLC, tabB[:, 0:LC_U], ALU.add)
            for (t0, n, v) in TOKBLOCKS:
                qb = t0 // 512
                keys = list(range(18)) if v == 0 else [16, 17]
                acc = (PS[4], PS[5])
                for idx, j in enumerate(keys):
                    ps = pss()
                    S.mm(ps[:, :n], kT[:, j * 128:(j + 1) * 128], qT[:, t0:t0 + n])
                    if v == 0 and j < 16:
                        u0 = 512 * qb - 128 * j + LL_OFF
                        msk = MLL[:, u0:u0 + n]
                    elif v == 0:
                        u0 = 512 * qb - 128 * (j - 16) + LC_OFF
                        msk = MLC[:, u0:u0 + n]
                    else:
                        u0 = -128 * (j - 16) + LL_OFF
                        msk = MLL[:, u0:u0 + n]
                    P = Pb()
                    S.tt(P[:, :n], ps[:, :n], msk, ALU.mult)
                    for c in range(2):
                        S.mm(acc[c][:, :n], V[:, j, c * 128:(c + 1) * 128], P[:, :n],
                             start=(idx == 0), stop=(idx == len(keys) - 1))
                S.dma(RG[:, :, :n], FM[32 + 2 * h:34 + 2 * h, :, t0:t0 + n].rearrange("c p t -> p c t"))
                pm = PS[6]
                for c in range(2):
                    s_ = sq()
                    S.act(s_[:, :n], acc[c][:, :n], AF.Square)
                    S.mm(pm[:, :n], ones, s_[:, :n], start=(c == 0), stop=(c == 1))
                S.act(rstd[:, :n], pm[:, :n], AF.Ln, scale=1.0 / 256, bias=epsc)
                S.act(rstd[:, :n], rstd[:, :n], AF.Exp, scale=-0.5)
                for c in range(2):
                    o = ob()
                    S.tt(o[:, :n], acc[c][:, :n], rstd[:, :n], ALU.mult)
                    S.tt(o[:, :n], o[:, :n], RG[:, c, :n], ALU.mult, eng="pool")
                    S.dma(MIX[8 + 2 * h + c, :, t0:t0 + n], o[:, :n], q="pool")
        A.release(mk)

    def post_stage(l, src, w_out, nmix, blocks):
        mk = A.mark()
        mixb = A.alloc(nmix, 512)
        xblk = A.alloc(16, 512)
        h2 = A.alloc(16, 512)
        wb = Rot([A.alloc(nmix, 128) for _ in range(2)])
        tmp2 = Rot([A.alloc(512) for _ in range(2)])
        rstd = A.alloc(512)
        wr = A.alloc(16, 36)
        br = A.alloc(36)
        lgt = A.alloc(36)
        m8 = A.alloc(8)
        oh = A.alloc(4)
        lgm = A.alloc(4, 8)
        eg = A.alloc(4)
        sm = A.alloc(1)
        wg = A.alloc(1)
        oh1 = A.alloc(32)
        oh2 = A.alloc(32)
        p1 = A.alloc(1)
        p2 = A.alloc(1)
        dlt = A.alloc(1)
        S.dma(wr, I["wr"][l])
        S.dma(br, I["br"][l])
        py = Rot(PS[1:5])
        for (t0, n, v) in blocks:
            S.dma(mixb[:, :, :n], MIX[0:nmix, :, t0:t0 + n].rearrange("c p t -> p c t"))
            S.dma(xblk[:, :, :n], src[:, :, t0:t0 + n].rearrange("c p t -> p c t"))
            for dc in range(16):
                w = wb()
                S.dma(w, w_out[:, dc * 128:(dc + 1) * 128].rearrange("(kc p) n -> p kc n", p=128))
                ps = py()
                for kc in range(nmix):
                    S.mm(ps[:, :n], w[:, kc, :], mixb[:, kc, :n], start=(kc == 0), stop=(kc == nmix - 1))
                S.stt(xblk[:, dc, :n], ps[:, :n], mod_ap(l, 2, dc, v), xblk[:, dc, :n], ALU.mult, ALU.add)
            S.dma(XT[:, :, t0:t0 + n].rearrange("c p t -> p c t"), xblk[:, :, :n], q="pool")
            rms_mod(xblk, n, lambda j: AN[l][:, 1, j, v:v + 1], lambda j: mod_ap(l, 3, j, v), h2, tmp2, rstd, PS[0])
            S.dma(H2T[:, :, t0:t0 + n].rearrange("c p t -> p c t"), h2[:, :, :n], q="pool")
            for tt_ in range(n // 128):
                ti = t0 // 128 + tt_
                pr = PS[5 + tt_ % 2]
                for kc in range(16):
                    S.mm(pr[:, 0:36], h2[:, kc, tt_ * 128:(tt_ + 1) * 128], wr[:, kc, :],
                         start=(kc == 0), stop=(kc == 15))
                S.tt(lgt, pr[:, 0:36], br, ALU.add)
                S.reduce(sm, lgt[:, 0:4], ALU.max)
                S.ts(oh, lgt[:, 0:4], sm, None, ALU.is_equal)
                S.ts(eg, lgt[:, 0:4], sm, None, ALU.subtract)
                S.act(eg, eg, AF.Exp)
                S.reduce(wg, eg, ALU.add)
                S.recip(wg, wg)
                S.ts(oh, oh, -1.0, 1.0e4, ALU.add, ALU.mult)
                for g in range(4):
                    S.ts(lgm[:, g, :], lgt[:, 4 + 8 * g:12 + 8 * g], oh[:, g:g + 1], None, ALU.add)
                lgf = lgm.rearrange("p a b -> p (a b)")
                S.reduce(m8[:, 0:1], lgf, ALU.max)
                S.ts(oh1, lgf, m8[:, 0:1], None, ALU.is_equal)
                S.stt(oh2, oh1, -1.0e4, lgf, ALU.mult, ALU.add)
                S.reduce(m8[:, 1:2], oh2, ALU.max)
                S.ts(oh2, oh2, m8[:, 1:2], None, ALU.is_equal)
                S.tt(dlt, m8[:, 1:2], m8[:, 0:1], ALU.subtract)
                S.act(dlt, dlt, AF.Exp)
                S.ts(p1, dlt, 1.0, None, ALU.add)
                S.recip(p1, p1)
                S.tt(p2, dlt, p1, ALU.mult)
                S.tt(G12[:, ti, 0:1], p1, wg, ALU.mult)
                S.tt(G12[:, ti, 1:2], p2, wg, ALU.mult)
                S.copy(OH1A[:, ti, :], oh1)
                S.copy(OH2A[:, ti, :], oh2)
        A.release(mk)

    def moe_stage(l, ntl, book_only=False, p1_only=False, p2_only=False, p3dbg=0):
        I32 = mybir.dt.int32
        NB = 2 * ntl + 32
        mk = A.mark()
        Mh = A.alloc(ntl, 32)
        CB = A.alloc(ntl, 32)
        RUN = A.alloc(32)
        PADC = A.alloc(32)
        PEND = A.alloc(32)
        PSTART = A.alloc(32)
        t32 = A.alloc(ntl, 32)
        DESTF = A.alloc(2, ntl)
        DESTI = A.alloc(2, ntl).bitcast(I32)
        BE = A.alloc(NB)
        IDXF = A.alloc(NB, 4)
        IDXI = A.alloc(NB, 4).bitcast(I32)
        slt = A.alloc(128)
        qp = A.alloc(4)
        S.dma(slt, I["sltT"])
        S.dma(qp, I["qp"])
        S.tt(Mh, OH1A[:, 0:ntl, :], OH2A[:, 0:ntl, :], ALU.add)
        S.memset(RUN, 0.0)
        pr = Rot([(PS[0], PS[1]), (PS[2], PS[3])])
        for i in range(ntl):
            pa, pb = pr()
            S.mm(pa[:, 0:32], slt, Mh[:, i, :])
            S.mm(pb[:, 0:32], ones, Mh[:, i, :])
            S.tt(CB[:, i, :], pa[:, 0:32], RUN, ALU.add)
            S.tt(RUN, pb[:, 0:32], RUN, ALU.add)
        S.memset(PADC, 0.0)
        for k in range(ntl):
            S.stt(PADC, RUN, float(128 * k), PADC, ALU.is_gt, ALU.add)
        S.ts(PADC, PADC, 128.0, None, ALU.mult)
        S.scan(PEND, ones[:, 0:32], PADC, 0.0, ALU.mult, ALU.add)
        S.tt(PSTART, PEND, PADC, ALU.subtract)
        for i in range(ntl):
            S.tt(CB[:, i, :], CB[:, i, :], PSTART, ALU.add)
        S.tt(t32, OH1A[:, 0:ntl, :], CB, ALU.mult)
        S.reduce(DESTF[:, 0, :], t32, ALU.add)
        S.tt(t32, OH2A[:, 0:ntl, :], CB, ALU.mult)
        S.reduce(DESTF[:, 1, :], t32, ALU.add)
        S.copy(DESTI, DESTF)
        for b in range(NB):
            S.ts(t32[:, 0, :], PEND, float(128 * b), None, ALU.is_le)
            S.reduce(BE[:, b:b + 1], t32[:, 0, :], ALU.add)
        S.ts(BE, BE, 31.0, None, ALU.min)
        S.ts(BE, BE, 512.0, float(l * 16384), ALU.mult, ALU.add)
        for b in range(NB):
            S.ts(IDXF[:, b, :], qp, BE[:, b:b + 1], None, ALU.add)
        S.copy(IDXI, IDXF)
        if book_only:
            S.dma(DBG[:, 2048:2048 + 2 * ntl], DESTF.rearrange("p a b -> p (a b)"), q="pool")
            S.dma(DBG[:, 2200:2200 + NB], BE, q="pool")
            S.dma(DBG[:, 2300:2332], PEND, q="pool")
            S.dma(DBG[:, 2340:2372], RUN, q="pool")
            S.dma(DBG[:, 2400:2400 + 4 * NB], IDXF.rearrange("p a b -> p (a b)"), q="pool")
            S.copy(t32.rearrange("p a b -> p (a b)")[:, 0:2 * ntl], DESTI.rearrange("p a b -> p (a b)"))
            S.dma(DBG[:, 2800:2800 + 2 * ntl], t32.rearrange("p a b -> p (a b)")[:, 0:2 * ntl], q="pool")
            A.release(mk)
            return
        mk1 = A.mark()
        zt = A.alloc(D)
        S.memset(zt, 0.0)
        for b in range(NB):
            S.dma(XS[b * 128:(b + 1) * 128, :], zt)
        hf = Rot([A.alloc(16, 128) for _ in range(2)])
        xtm = Rot([A.alloc(D) for _ in range(2)])
        pt4 = Rot(PS[4:8])
        for i in range(ntl):
            h = hf()
            S.dma(h, H2T[:, :, i * 128:(i + 1) * 128].rearrange("c p t -> p c t"))
            xt = xtm()
            for g4 in range(4):
                ps = pt4()
                for c in range(4):
                    S.transpose(ps[:, c * 128:(c + 1) * 128], h[:, g4 * 4 + c, :], ident)
                S.copy(xt[:, g4 * 512:(g4 + 1) * 512], ps, eng="act" if g4 % 2 else "dve")
            for k in range(2):
                idx = DESTI[:, k, i:i + 1]
                S.dma_fn(lambda e, xt=xt, idx=idx: e.indirect_dma_start(
                    out=XS[:, :], out_offset=bass.IndirectOffsetOnAxis(ap=idx, axis=0), in_=xt, in_offset=None),
                    [xt, idx], [XS], q="pool")
        A.release(mk1)
        if p1_only:
            A.release(mk)
            return
        xb = Rot([A.alloc(D) for _ in range(2)])
        xbT = A.alloc(16, 128)
        w13 = Rot([A.alloc(16, 256) for _ in range(5)])
        w2b = Rot([A.alloc(8, 512) for _ in range(2)])
        actm = A.alloc(1024)
        tmpa = Rot([A.alloc(256) for _ in range(2)])
        actT = A.alloc(8, 128)
        yb = Rot([A.alloc(D) for _ in range(2)])
        W1 = I["moe_w1"]
        W3 = I["moe_w3"]
        W2 = I["moe_w2"]

        def gather(dst, src, idx, dep_src=False):
            S.dma_fn(lambda e: e.indirect_dma_start(
                out=dst, out_offset=None, in_=src[:, :], in_offset=bass.IndirectOffsetOnAxis(ap=idx, axis=0)),
                [idx, src] if dep_src else [idx], [dst], q="pool", max_inflight=2)
        for b in range(NB):
            x_ = xb()
            S.dma(x_, XS[b * 128:(b + 1) * 128, :])
            for g4 in range(4):
                ps = pt4()
                for c in range(4):
                    S.transpose(ps[:, c * 128:(c + 1) * 128], x_[:, (g4 * 4 + c) * 128:(g4 * 4 + c + 1) * 128], ident)
                S.copy(xbT[:, g4 * 4:(g4 + 1) * 4, :].rearrange("p a b -> p (a b)"), ps, eng="act" if g4 % 2 else "dve")
            pa2 = (PS[0], PS[1])
            pb2 = (PS[2], PS[3])
            for q in range(4):
                idx = IDXI[:, b, q:q + 1]
                w1p = w13()
                gather(w1p.rearrange("p a b -> p (a b)"), W1, idx)
                w3p = w13()
                gather(w3p.rearrange("p a b -> p (a b)"), W3, idx)
                oa = pa2[q // 2][:, (q % 2) * 256:(q % 2 + 1) * 256]
                ob_ = pb2[q // 2][:, (q % 2) * 256:(q % 2 + 1) * 256]
                for kc in range(16):
                    S.mm(oa, xbT[:, kc, :], w1p[:, kc, :], start=(kc == 0), stop=(kc == 15))
                for kc in range(16):
                    S.mm(ob_, xbT[:, kc, :], w3p[:, kc, :], start=(kc == 0), stop=(kc == 15))
                t = tmpa()
                S.act(t, oa, AF.Silu)
                S.tt(actm[:, q * 256:(q + 1) * 256], t, ob_, ALU.mult)
            for g2 in range(2):
                ps = pt4()
                for c in range(4):
                    fc = g2 * 4 + c
                    S.transpose(ps[:, c * 128:(c + 1) * 128], actm[:, fc * 128:(fc + 1) * 128], ident)
                S.copy(actT[:, g2 * 4:(g2 + 1) * 4, :].rearrange("p a b -> p (a b)"), ps, eng="act" if g2 % 2 else "dve")
            y_ = yb()
            for cb in range(4):
                w2p = w2b()
                gather(w2p.rearrange("p a b -> p (a b)"), W2, IDXI[:, b, cb:cb + 1])
                ps = pt4()
                for fc in range(8):
                    S.mm(ps, actT[:, fc, :], w2p[:, fc, :], start=(fc == 0), stop=(fc == 7))
                S.copy(y_[:, cb * 512:(cb + 1) * 512], ps, eng="act" if cb % 2 else "dve")
            S.dma(YB[b * 128:(b + 1) * 128, :], y_)
        A.release(mk1)
        if p2_only:
            A.release(mk)
            return
        y1 = Rot([A.alloc(D) for _ in range(2)])
        y2 = Rot([A.alloc(D) for _ in range(2)])
        for i in range(ntl):
            v = 0 if i < 16 else 1
            a1 = y1()
            a2 = y2()
            gather(a1, YB, DESTI[:, 0, i:i + 1], dep_src=True)
            gather(a2, YB, DESTI[:, 1, i:i + 1], dep_src=True)
            S.ts(a1, a1, G12[:, i, 0:1], None, ALU.mult)
            S.stt(a1, a2, G12[:, i, 1:2], a1, ALU.mult, ALU.add)
            if p3dbg:
                if i < 2:
                    S.dma(DBG[:, i * 2048:(i + 1) * 2048], a1, q="sp")
                if i + 1 >= p3dbg:
                    break
                continue
            S.dma(XS[i * 128:(i + 1) * 128, :], a1)
        A.release(mk1)
        acc = A.alloc(4, 2048)
        xblk = A.alloc(16, 512)
        blocks = TOKBLOCKS if ntl == 18 else TOKBLOCKS[:4]
        for (t0, n, v) in blocks:
            nt_ = n // 128
            for tt_ in range(nt_):
                S.dma(acc[:, tt_, :], XS[t0 + tt_ * 128:t0 + (tt_ + 1) * 128, :])
            S.dma(xblk[:, :, :n], XT[:, :, t0:t0 + n].rearrange("c p t -> p c t"))
            for dc in range(16):
                pt = pt4()
                for tt_ in range(nt_):
                    S.transpose(pt[:, tt_ * 128:(tt_ + 1) * 128], acc[:, tt_, dc * 128:(dc + 1) * 128], ident)
                S.stt(xblk[:, dc, :n], pt[:, :n], mod_ap(l, 5, dc, v), xblk[:, dc, :n], ALU.mult, ALU.add)
            S.dma(XT[:, :, t0:t0 + n].rearrange("c p t -> p c t"), xblk[:, :, :n], q="pool")
        A.release(mk)

    def diff_stage():
        import math
        lam_init = 0.8 - 0.6 * math.exp(-0.3 * 1)
        mk = A.mark()
        qT = A.alloc(NLAT)
        kT = A.alloc(NT)
        V = A.alloc(18, 128)
        OUT = A.alloc(NLAT)
        dl = A.alloc(4, 64)
        pr = A.alloc(2, 64)
        lam = A.alloc(2)
        nlam = A.alloc(1)
        Pb = Rot([A.alloc(512) for _ in range(3)])
        r0 = A.alloc(512)
        o0 = A.alloc(512)
        o1 = A.alloc(512)
        sq = A.alloc(512)
        rstd = A.alloc(512)
        S.dma(dl, I["dlam"])
        S.tt(pr[:, 0, :], dl[:, 0, :], dl[:, 1, :], ALU.mult)
        S.tt(pr[:, 1, :], dl[:, 2, :], dl[:, 3, :], ALU.mult)
        S.reduce(lam[:, 0:1], pr[:, 0, :], ALU.add)
        S.reduce(lam[:, 1:2], pr[:, 1, :], ALU.add)
        S.act(lam, lam, AF.Exp)
        S.tt(nlam, lam[:, 1:2], lam[:, 0:1], ALU.subtract)
        S.ts(nlam, nlam, -lam_init, None, ALU.add)
        pss = Rot(PS[0:4])
        for h in range(8):
            S.dma(qT, FM[h, :, 0:NLAT])
            S.dma(kT, FM[8 + h])
            S.dma(V, TM[:, h * 128:(h + 1) * 128].rearrange("(j p) d -> p j d", p=128))
            for qb in range(4):
                q0 = qb * 512
                for c in range(2):
                    O = PS[4 + c]
                    Z = PS[6 + c]
                    for j in range(18):
                        ps = pss()
                        S.mm(ps, kT[c * 64:(c + 1) * 64, j * 128:(j + 1) * 128], qT[c * 64:(c + 1) * 64, q0:q0 + 512])
                        P = Pb()
                        S.act(P, ps, AF.Exp)
                        S.mm(O, V[:, j, :], P, start=(j == 0), stop=(j == 17))
                        S.mm(Z, ones, P, start=(j == 0), stop=(j == 17))
                S.recip(r0, PS[6])
                S.tt(o0, PS[4], r0, ALU.mult)
                S.recip(r0, PS[7])
                S.tt(o1, PS[5], r0, ALU.mult)
                S.stt(o0, o1, nlam, o0, ALU.mult, ALU.add)
                S.act(sq, o0, AF.Square)
                pm = pss()
                S.mm(pm, ones, sq)
                S.act(rstd, pm, AF.Ln, scale=1.0 / 128, bias=epsc)
                S.act(rstd, rstd, AF.Exp, scale=-0.5)
                S.stt(OUT[:, q0:q0 + 512], o0, 1.0 - lam_init, rstd, ALU.mult, ALU.mult)
            S.dma(MIX[h, :, 0:NLAT], OUT, q="pool")
        A.release(mk)

    def hgrn_stage():
        mk = A.mark()
        qT = A.alloc(NLAT)
        Kd = [A.alloc(NT), A.alloc(NT)]
        Gd = [A.alloc(NT), A.alloc(NT)]
        NGd = [A.alloc(NT), A.alloc(NT)]
        V = A.alloc(18, 128)
        onesr = A.alloc(NT)
        OUT = A.alloc(NLAT)
        LF = A.alloc(NT)
        Kt = [A.alloc(NT), A.alloc(NT)]
        Dt = [A.alloc(NT), A.alloc(NT)]
        eq = Rot([A.alloc(32) for _ in range(2)])
        Qt = [Rot([A.alloc(32) for _ in range(2)]), Rot([A.alloc(32) for _ in range(2)])]
        Pb = Rot([A.alloc(21, 32) for _ in range(2)])
        trilT = A.alloc(128)
        triuT = A.alloc(128)
        HG = A.alloc(512)
        sq = A.alloc(512)
        rstd = A.alloc(512)
        ob = Rot([A.alloc(512) for _ in range(2)])
        S.dma(trilT, I["trilT"])
        S.dma(triuT, I["triuT"])
        S.memset(onesr, 1.0)
        pscore = Rot([(PS[0], PS[1]), (PS[2], PS[3])])
        pout = Rot(PS[4:6])
        for h in range(8):
            S.dma(qT, FM[16 + h, :, 0:NLAT])
            S.dma(V, TM[:, 1024 + h * 128:1024 + (h + 1) * 128].rearrange("(j p) d -> p j d", p=128))
            S.dma(Kd[0], FM[24 + h])
            S.dma(LF, FM[32 + h])
            S.scan(Gd[0][:, NLAT:NT], onesr[:, 0:NCTX], LF[:, NLAT:NT], 0.0, ALU.mult, ALU.add)
            S.scan(Gd[0][:, 0:NLAT], onesr[:, 0:NLAT], LF[:, 0:NLAT], Gd[0][:, NT - 1:NT], ALU.mult, ALU.add)
            S.ts(NGd[0], Gd[0], -1.0, None, ALU.mult)
            S.dma(Kd[1], FM[40 + h])
            S.dma(LF, FM[48 + h])
            S.scan(Gd[1], onesr, LF, 0.0, ALU.mult, ALU.add)
            S.tt(Gd[1], LF, Gd[1], ALU.subtract)
            S.ts(NGd[1], Gd[1], -1.0, None, ALU.mult)
            for sb in range(64):
                t0 = 32 * sb
                Ii = t0 // 128
                sbi = sb % 4
                kblks = []
                qts = []
                for d in range(2):
                    if d == 0:
                        ai = t0 - 1 if t0 > 0 else NT - 1
                        slices = [(0, (Ii + 1) * 128), (NLAT, NT)]
                        tl = [16, 17] + list(range(0, Ii + 1))
                    else:
                        ai = t0 + 32
                        slices = [(Ii * 128, NT)]
                        tl = list(range(Ii, 16)) + [16, 17]
                    a = Gd[d][:, ai:ai + 1]
                    na = NGd[d][:, ai:ai + 1]
                    e_ = eq()
                    S.act(e_, Gd[d][:, t0:t0 + 32], AF.Exp, bias=na, scale=1.0)
                    q_ = Qt[d]()
                    S.tt(q_, qT[:, t0:t0 + 32], e_, ALU.mult)
                    qts.append(q_)
                    for (lo, hi) in slices:
                        S.ts(Dt[d][:, lo:hi], Gd[d][:, lo:hi], a, -60.0, ALU.subtract, ALU.max)
                        S.act(Dt[d][:, lo:hi], Dt[d][:, lo:hi], AF.Exp, scale=-1.0)
                        S.tt(Kt[d][:, lo:hi], Kd[d][:, lo:hi], Dt[d][:, lo:hi], ALU.mult, eng="pool")
                    kblks += [(d, j) for j in tl]
                assert len(kblks) == 21
                banks = pscore()
                for k, (d, j) in enumerate(kblks):
                    S.mm(banks[k // 16][:, (k % 16) * 32:(k % 16 + 1) * 32], Kt[d][:, j * 128:(j + 1) * 128], qts[d])
                P = Pb()
                Pf = P.rearrange("p a b -> p (a b)")
                S.copy(Pf[:, 0:512], banks[0][:, 0:512], eng="act")
                S.copy(Pf[:, 512:672], banks[1][:, 0:160], eng="act")
                kf = 2 + Ii
                kb = (Ii + 3)
                S.tt(P[:, kf, :], P[:, kf, :], trilT[:, 32 * sbi:32 * sbi + 32], ALU.mult)
                S.tt(P[:, kb, :], P[:, kb, :], triuT[:, 32 * sbi:32 * sbi + 32], ALU.mult)
                po = pout()
                for k, (d, j) in enumerate(kblks):
                    S.mm(po[:, 0:32], V[:, j, :], P[:, k, :], start=(k == 0), stop=(k == 20))
                S.copy(OUT[:, t0:t0 + 32], po[:, 0:32], eng="act")
            for qb in range(4):
                q0 = qb * 512
                S.dma(HG, FM[56 + h, :, q0:q0 + 512])
                S.act(sq, OUT[:, q0:q0 + 512], AF.Square)
                pm = PS[6 + qb % 2]
                S.mm(pm, ones, sq)
                S.act(rstd, pm, AF.Ln, scale=1.0 / 128, bias=epsc)
                S.act(rstd, rstd, AF.Exp, scale=-0.5)
                o = ob()
                S.tt(o, OUT[:, q0:q0 + 512], rstd, ALU.mult)
                S.tt(o, o, HG, ALU.mult)
                S.dma(MIX[8 + h, :, q0:q0 + 512], o, q="pool")
        A.release(mk)

    def final_stage():
        mk = A.mark()
        xblk = A.alloc(16, 512)
        ob = A.alloc(16, 512)
        tmp2 = Rot([A.alloc(512) for _ in range(2)])
        rstd = A.alloc(512)
        for (t0, n, v) in TOKBLOCKS[:4]:
            S.dma(xblk, XT[:, :, t0:t0 + n].rearrange("c p t -> p c t"))
            rms_mod(xblk, n, lambda j: NFW[:, j:j + 1], None, ob, tmp2, rstd, PS[0])
            S.dma(outT[:, :, t0:t0 + n].rearrange("c p t -> p c t"), ob, q="pool", is_output=True)
        A.release(mk)

    ALL = ["adaln0", "inproj0", "na0", "ret0", "post0", "moe0", "adaln1", "inproj1", "diff1", "hgrn1", "post1", "moe1", "final"]
    st = stages or ALL
    FM0 = [(0, 8, 0, "scale", 128 ** -0.5), (1024, 8, 8, "scale", 1.0), (3072, 8, 16, "rope", 1.0),
           (4096, 8, 24, "rope", 128 ** -0.5), (7168, 16, 32, "silu", None)]
    TM0 = [(2048, 1024, 0), (5120, 2048, 1024)]
    FM1 = [(0, 8, 0, "rope", 64 ** -0.5), (1024, 8, 8, "rope", 1.0), (3072, 8, 16, "silu", None),
           (4096, 8, 24, "gate", 32), (5120, 8, 40, "gate", 48), (7168, 8, 56, "silu", None)]
    TM1 = [(2048, 1024, 0), (6144, 1024, 1024)]
    if "adaln0" in st:
        adaln(0)
    if "inproj0" in st:
        inproj(0, I["xin"], I["w_in_even"], FM0, TM0, ("cos128", "sin128", "perm128"))
    if "na0" in st:
        na_stage()
    if "ret0" in st:
        ret_stage()
    if "post0" in st:
        post_stage(0, I["xin"], I["w_out_even"], 24, TOKBLOCKS)
    if "dbggates" in st:
        S.dma(DBG[:, 256:256 + 576], OH1A.rearrange("p a b -> p (a b)"), q="pool")
    if "moe0book" in st:
        moe_stage(0, 18, book_only=True)
    if "moe0p1" in st:
        moe_stage(0, 18, p1_only=True)
    if "moe0p3a" in st:
        moe_stage(0, 18, p3dbg=1)
    if "moe0p3b" in st:
        moe_stage(0, 18, p3dbg=18)
    if "moe0p2" in st:
        moe_stage(0, 18, p2_only=True)
    if "moe0" in st:
        moe_stage(0, 18)
    if "adaln1" in st:
        adaln(1)
    if "inproj1" in st:
        inproj(1, XT, I["w_in_odd"], FM1, TM1, ("cos64", "sin64", "perm64"))
    if "diff1" in st:
        diff_stage()
    if "hgrn1" in st:
        hgrn_stage()
    if "post1" in st:
        post_stage(1, XT, I["w_out_odd"], 16, TOKBLOCKS[:4])
    if "dbggates1" in st:
        S.dma(DBG[:, 1024:1024 + 576], OH1A.rearrange("p a b -> p (a b)"), q="pool")
    if "moe1" in st:
        moe_stage(1, 16)
    if "final" in st:
        final_stage()
    if "dbgmod" in st:
        S.dma(DBG[:, 0:192], MOD[0].rearrange("p a b -> p (a b)"), q="pool")
        S.dma(DBG[:, 192:256], AN[0].rearrange("p a b c -> p (a b c)"), q="pool")
    S.emit()
    stack.close()
    return nc


def kernel(**inputs):
    maps = host_prep(inputs)
    nc = build_program()
    res = run_bass_kernel_spmd(nc, maps, core_ids=list(range(8)))
    outs = []
    for r in res.results:
        o = np.asarray(r["outT"], dtype=np.float32).reshape(D, NLAT)
        outs.append(o.T)
    return np.ascontiguousarray(np.stack(outs, axis=0), dtype=np.float32)
```
